# Optimizing a Trainium2 kernel written in Bass

```python
import math
import numpy as np
import jax
import jax.numpy as jnp
from jax import lax

D_MODEL = 1024
BATCH = 2
SEQ = 16384
DEPTH = 4

CTX_LEN = 256
GRID_W = 64
DA_HEADS = 6
DA_DIM = 32
DA_VDIM = 2 * DA_DIM
MLA_HEADS = 6
MLA_NOPE = 64
MLA_ROPE = 32
MLA_VDIM = 64
MLA_Q_RANK = 256
MLA_KV_RANK = 128
RET_HEADS = 4
RET_DIM = 64
RET_CHUNK = 128
DA_WIDTH = DA_HEADS * DA_VDIM
MLA_WIDTH = MLA_HEADS * MLA_VDIM
RET_WIDTH = RET_HEADS * RET_DIM
MIX_WIDTH = DA_WIDTH + MLA_WIDTH + RET_WIDTH
IN_SPLIT_SIZES = (DA_HEADS * 2 * DA_DIM, DA_HEADS * 2 * DA_DIM, DA_HEADS * DA_VDIM,
                  MLA_Q_RANK, MLA_KV_RANK, MLA_ROPE,
                  RET_WIDTH, RET_WIDTH, RET_WIDTH, RET_WIDTH)
IN_COLS = sum(IN_SPLIT_SIZES)
ATTN_ROT_DIM = 32
ROPE_BASE = 10000.0
Q_BLOCK = 128
N_EXPERTS = 16
N_GROUPS = 4
EXPERTS_PER_GROUP = N_EXPERTS // N_GROUPS
TOPK_GROUP = 1
TOP_K = 2
D_EXPERT = 512
ROUTED_SCALE = 1.0
N_MOD = 6
ALPHA = (2 * DEPTH) ** 0.25
BETA = (8 * DEPTH) ** -0.25
LN_EPS = 1e-5
RMS_EPS = 1e-6

kernel_name = 'hybrid_diffattn_mla_retention_grouped_moe'


def layer_norm(x, g, b):
    xf = x.astype(jnp.float32)
    mu = jnp.mean(xf, -1, keepdims=True)
    var = jnp.mean(jnp.square(xf - mu), -1, keepdims=True)
    return ((xf - mu) * lax.rsqrt(var + LN_EPS)).astype(x.dtype) * g + b


def rms_norm(x, g=None):
    xf = x.astype(jnp.float32)
    y = (xf * lax.rsqrt(jnp.mean(jnp.square(xf), -1, keepdims=True) + RMS_EPS)).astype(x.dtype)
    return y if g is None else y * g


def axial_rope(rows, rot_dim):
    r = jnp.repeat(jnp.arange(rows, dtype=jnp.float32), GRID_W)
    col = jnp.tile(jnp.arange(GRID_W, dtype=jnp.float32), rows)
    n_freq = rot_dim // 4
    freqs = ROPE_BASE ** (-jnp.arange(n_freq, dtype=jnp.float32) / n_freq)
    ang = jnp.concatenate([r[:, None] * freqs, col[:, None] * freqs], -1)
    return jnp.cos(ang), jnp.sin(ang)


def apply_rope(x, rope):
    cos, sin = rope
    cos = cos.astype(x.dtype)
    sin = sin.astype(x.dtype)
    x1, x2 = x[..., 0::2], x[..., 1::2]
    return jnp.stack([x1 * cos - x2 * sin, x1 * sin + x2 * cos], -1).reshape(x.shape)


def split_cols(z):
    return jnp.split(z, np.cumsum(IN_SPLIT_SIZES)[:-1].tolist(), axis=-1)


def split_heads(t, n_heads):
    B, L, _ = t.shape
    return t.reshape(B, L, n_heads, -1).transpose(0, 2, 1, 3)


def merge_heads(t):
    B, H, L, d = t.shape
    return t.transpose(0, 2, 1, 3).reshape(B, L, H * d)


def attend_plain(q, k, v, scale):
    s = jnp.einsum('bhmqd,bhmkd->bhmqk', q, k).astype(jnp.float32) * scale
    p = jax.nn.softmax(s, -1).astype(v.dtype)
    return jnp.einsum('bhmqk,bhkd->bhmqd', p, v)


def attend_latent(q_rot, q_free, k_lat, k_ctx, v_lat, v_ctx, scale):
    B, H, M, S, _ = q_rot.shape
    n_lat = k_lat.shape[3]

    def block(i):
        start = i * Q_BLOCK
        qr = lax.dynamic_slice_in_dim(q_rot, start, Q_BLOCK, axis=3)
        qf = lax.dynamic_slice_in_dim(q_free, start, Q_BLOCK, axis=3)
        s = jnp.concatenate([jnp.einsum('bhmqd,bhmkd->bhmqk', qr, k_lat),
                             jnp.einsum('bhmqd,bhmkd->bhmqk', qf, k_ctx)], -1)
        p = jax.nn.softmax(s.astype(jnp.float32) * scale, -1).astype(v_lat.dtype)
        return (jnp.einsum('bhmqk,bhkd->bhmqd', p[..., :n_lat], v_lat)
                + jnp.einsum('bhmqk,bhkd->bhmqd', p[..., n_lat:], v_ctx))

    out = lax.map(block, jnp.arange(S // Q_BLOCK))
    return jnp.moveaxis(out, 0, 3).reshape(B, H, M, S, v_lat.shape[-1])


def diff_attention(lat, ctx_in, lam_vecs, subln_g, layer_idx, rope, last):
    def shape_qk(z):
        B, L, _ = z.shape
        return z.reshape(B, L, DA_HEADS, 2, DA_DIM).transpose(0, 2, 3, 1, 4)
    q, k, v = shape_qk(lat[0]), shape_qk(lat[1]), split_heads(lat[2], DA_HEADS)
    qc, kc, vc = shape_qk(ctx_in[0]), shape_qk(ctx_in[1]), split_heads(ctx_in[2], DA_HEADS)
    lam_init = 0.8 - 0.6 * math.exp(-0.3 * layer_idx)
    lf = lam_vecs.astype(jnp.float32)
    lam = jnp.exp(jnp.sum(lf[0] * lf[1])) - jnp.exp(jnp.sum(lf[2] * lf[3])) + lam_init
    scale = DA_DIM ** -0.5

    def combine(o):
        o = o[:, :, 0] - lam.astype(o.dtype) * o[:, :, 1]
        return merge_heads(rms_norm(o, subln_g) * (1.0 - lam_init))

    y = combine(attend_latent(apply_rope(q, rope), q, apply_rope(k, rope), kc, v, vc, scale))
    yc = None if last else combine(attend_plain(qc, kc, vc, scale))
    return y, yc


def mla_attention(lat, ctx_in, q_norm_g, w_uq, kv_norm_g, w_ukv, rope, last):
    def project(cq, ckv, kpe):
        B, L, _ = cq.shape
        q = (rms_norm(cq, q_norm_g) @ w_uq).reshape(B, L, MLA_HEADS, MLA_NOPE + MLA_ROPE)
        q = q.transpose(0, 2, 1, 3)[:, :, None]
        kv = (rms_norm(ckv, kv_norm_g) @ w_ukv).reshape(B, L, MLA_HEADS, MLA_NOPE + MLA_VDIM)
        kv = kv.transpose(0, 2, 1, 3)
        return q, kv[:, :, None, :, :MLA_NOPE], kpe[:, None, None], kv[..., MLA_NOPE:]
    q, k_nope, k_pe, v = project(*lat)
    qc, kc_nope, kc_pe, vc = project(*ctx_in)

    def full_k(kn, kp):
        return jnp.concatenate([kn, jnp.broadcast_to(kp, kn.shape[:-1] + (MLA_ROPE,))], -1)
    q_rot = jnp.concatenate([q[..., :MLA_NOPE], apply_rope(q[..., MLA_NOPE:], rope)], -1)
    k_lat = full_k(k_nope, apply_rope(k_pe, rope))
    k_ctx = full_k(kc_nope, kc_pe)
    scale = (MLA_NOPE + MLA_ROPE) ** -0.5
    y = merge_heads(attend_latent(q_rot, q, k_lat, k_ctx, v, vc, scale)[:, :, 0])
    yc = None if last else merge_heads(attend_plain(qc, k_ctx, vc, scale)[:, :, 0])
    return y, yc


def chunk_retention(q, k, v, log_gamma, state0, strict, with_output=True):
    B, H, L, dk = q.shape
    dv = v.shape[-1]
    n_chunks = L // RET_CHUNK
    qb = q.reshape(B, H, n_chunks, RET_CHUNK, dk)
    kb = k.reshape(B, H, n_chunks, RET_CHUNK, dk)
    vb = v.reshape(B, H, n_chunks, RET_CHUNK, dv)
    pos = jnp.arange(RET_CHUNK, dtype=jnp.float32)
    lg = log_gamma[:, None]
    zeta = jnp.exp(lg * (RET_CHUNK - 1 - pos)).astype(q.dtype)
    chunk_decay = jnp.exp(log_gamma * RET_CHUNK).astype(q.dtype)[None, :, None, None]
    upd = jnp.einsum('bhncd,bhnce->bhnde', kb * zeta[None, :, None, :, None], vb)

    def step(state, u):
        return state * chunk_decay + u, state
    final, prev = lax.scan(step, state0, jnp.moveaxis(upd, 2, 0))
    if not with_output:
        return None, final
    diff = pos[:, None] - pos[None, :]
    mask = diff > 0 if strict else diff >= 0
    dmask = jnp.where(mask[None], jnp.exp(lg[..., None] * jnp.where(mask, diff, 0.0)[None]), 0.0).astype(q.dtype)
    xi = jnp.exp(lg * (pos + 1)).astype(q.dtype)
    a = jnp.einsum('bhncd,bhnmd->bhncm', qb, kb) * dmask[None, :, None]
    inner = jnp.einsum('bhncm,bhnme->bhnce', a, vb)
    cross = jnp.einsum('bhncd,nbhde->bhnce', qb * xi[None, :, None, :, None], prev)
    return (inner + cross).reshape(B, H, L, dv), final


def retention(lat, ctx_in, decay_f, decay_b, rope, last):
    zq, zk, zv, zg = lat
    zqc, zkc, zvc, zgc = ctx_in
    kscale = RET_DIM ** -0.5
    q = apply_rope(split_heads(zq, RET_HEADS), rope)
    k = apply_rope(split_heads(zk, RET_HEADS), rope) * kscale
    v = split_heads(zv, RET_HEADS)
    qc = split_heads(zqc, RET_HEADS)
    kc = split_heads(zkc, RET_HEADS) * kscale
    vc = split_heads(zvc, RET_HEADS)
    lg_f = jax.nn.log_sigmoid(decay_f.astype(jnp.float32))
    lg_b = jax.nn.log_sigmoid(decay_b.astype(jnp.float32))
    zero = jnp.zeros((q.shape[0], RET_HEADS, RET_DIM, RET_DIM), q.dtype)

    def flip(t):
        return jnp.flip(t, axis=2)
    oc_f, st_f = chunk_retention(qc, kc, vc, lg_f, zero, False, with_output=not last)
    oc_b, st_b = chunk_retention(flip(qc), flip(kc), flip(vc), lg_b, zero, True, with_output=not last)
    o_f, _ = chunk_retention(q, k, v, lg_f, st_f, False)
    o_b, _ = chunk_retention(flip(q), flip(k), flip(v), lg_b, st_b, True)

    def finish(o, zgate):
        return jax.nn.silu(zgate) * merge_heads(rms_norm(o))
    y = finish(o_f + flip(o_b), zg)
    yc = None if last else finish(oc_f + flip(oc_b), zgc)
    return y, yc


def moe(tokens, router_w, router_b, w1, w3, w2):
    T = tokens.shape[0]
    scores = jax.nn.sigmoid((tokens @ router_w).astype(jnp.float32))
    choice = scores + router_b.astype(jnp.float32)
    group_score = lax.top_k(choice.reshape(T, N_GROUPS, EXPERTS_PER_GROUP), 2)[0].sum(-1)
    _, gidx = lax.top_k(group_score, TOPK_GROUP)
    gmask = jax.nn.one_hot(gidx, N_GROUPS, dtype=jnp.float32).sum(1)
    emask = jnp.repeat(gmask, EXPERTS_PER_GROUP, axis=-1)
    _, eidx = lax.top_k(jnp.where(emask > 0, choice, -jnp.inf), TOP_K)
    wsel = jnp.take_along_axis(scores, eidx, -1)
    wsel = wsel / jnp.sum(wsel, -1, keepdims=True) * ROUTED_SCALE
    gates = jnp.sum(jax.nn.one_hot(eidx, N_EXPERTS, dtype=jnp.float32) * wsel[..., None], 1).astype(tokens.dtype)

    def expert_step(acc, p):
        w1e, w3e, w2e, ge = p
        y = (jax.nn.silu(tokens @ w1e) * (tokens @ w3e)) @ w2e
        return acc + ge[:, None] * y, None
    out, _ = lax.scan(expert_step, jnp.zeros_like(tokens), (w1, w3, w2, gates.T))
    return out


def setup_inputs(seed: int = 0) -> dict:
    key = jax.random.key(seed)
    ks = jax.random.split(key, 25)
    f32 = jnp.float32

    def nrm(k, shape, s):
        return jax.random.normal(k, shape, f32) * s

    def gain(k, shape):
        return 1.0 + nrm(k, shape, 0.02)
    decay_init = jnp.log(2.0 ** (5.0 + jnp.arange(RET_HEADS, dtype=f32)) - 1.0)
    return {
        'x': nrm(ks[0], (BATCH, SEQ, D_MODEL), 1.0),
        'c': nrm(ks[1], (BATCH, D_MODEL), 1.0),
        'ctx': nrm(ks[2], (BATCH, CTX_LEN, D_MODEL), 1.0),
        'c_ctx': nrm(ks[3], (D_MODEL,), 1.0),
        'w_ada': nrm(ks[4], (DEPTH, D_MODEL, N_MOD * D_MODEL), 0.5 * D_MODEL ** -0.5),
        'b_ada': nrm(ks[5], (DEPTH, N_MOD * D_MODEL), 0.02),
        'w_in': nrm(ks[6], (DEPTH, D_MODEL, IN_COLS), D_MODEL ** -0.5),
        'da_lambda': nrm(ks[7], (DEPTH, 4, DA_DIM), 0.1),
        'da_subln': gain(ks[8], (DEPTH, DA_VDIM)),
        'mla_q_norm': gain(ks[9], (DEPTH, MLA_Q_RANK)),
        'mla_w_uq': nrm(ks[10], (DEPTH, MLA_Q_RANK, MLA_HEADS * (MLA_NOPE + MLA_ROPE)), MLA_Q_RANK ** -0.5),
        'mla_kv_norm': gain(ks[11], (DEPTH, MLA_KV_RANK)),
        'mla_w_ukv': nrm(ks[12], (DEPTH, MLA_KV_RANK, MLA_HEADS * (MLA_NOPE + MLA_VDIM)), MLA_KV_RANK ** -0.5),
        'ret_decay_f': decay_init + nrm(ks[13], (DEPTH, RET_HEADS), 0.1),
        'ret_decay_b': decay_init + nrm(ks[14], (DEPTH, RET_HEADS), 0.1),
        'w_out': nrm(ks[15], (DEPTH, MIX_WIDTH, D_MODEL), BETA * MIX_WIDTH ** -0.5),
        'ln1_g': gain(ks[16], (DEPTH, D_MODEL)),
        'ln1_b': nrm(ks[17], (DEPTH, D_MODEL), 0.02),
        'router_w': nrm(ks[18], (D_MODEL, N_EXPERTS), D_MODEL ** -0.5),
        'router_b': nrm(ks[19], (N_EXPERTS,), 0.01),
        'exp_w1': nrm(ks[20], (DEPTH, N_EXPERTS, D_MODEL, D_EXPERT), D_MODEL ** -0.5),
        'exp_w3': nrm(ks[21], (DEPTH, N_EXPERTS, D_MODEL, D_EXPERT), D_MODEL ** -0.5),
        'exp_w2': nrm(ks[22], (DEPTH, N_EXPERTS, D_EXPERT, D_MODEL), BETA * D_EXPERT ** -0.5),
        'ln2_g': gain(ks[23], (DEPTH, D_MODEL)),
        'ln2_b': nrm(ks[24], (DEPTH, D_MODEL), 0.02),
    }


def reference(x, c, ctx, c_ctx, w_ada, b_ada, w_in, da_lambda, da_subln, mla_q_norm, mla_w_uq,
              mla_kv_norm, mla_w_ukv, ret_decay_f, ret_decay_b, w_out, ln1_g, ln1_b,
              router_w, router_b, exp_w1, exp_w3, exp_w2, ln2_g, ln2_b):
    B, S, D = x.shape
    C = ctx.shape[1]
    ROWS = S // GRID_W
    rope_attn = axial_rope(ROWS, ATTN_ROT_DIM)
    rope_ret = axial_rope(ROWS, RET_DIM)
    silu_c = jax.nn.silu(c)
    silu_cc = jax.nn.silu(c_ctx)
    for l in range(DEPTH):
        last = l == DEPTH - 1
        mod = (silu_c @ w_ada[l] + b_ada[l]).reshape(B, N_MOD, 1, D)
        modc = (silu_cc @ w_ada[l] + b_ada[l]).reshape(N_MOD, D)
        h = x * (1 + mod[:, 1]) + mod[:, 0]
        hc = ctx * (1 + modc[1]) + modc[0]
        parts = split_cols(h @ w_in[l])
        parts_c = split_cols(hc @ w_in[l])
        y_da, yc_da = diff_attention(parts[0:3], parts_c[0:3], da_lambda[l], da_subln[l], l, rope_attn, last)
        y_mla, yc_mla = mla_attention(parts[3:6], parts_c[3:6], mla_q_norm[l], mla_w_uq[l],
                                      mla_kv_norm[l], mla_w_ukv[l], rope_attn, last)
        y_ret, yc_ret = retention(parts[6:10], parts_c[6:10], ret_decay_f[l], ret_decay_b[l], rope_ret, last)
        y = jnp.concatenate([y_da, y_mla, y_ret], -1) @ w_out[l]
        x = layer_norm(ALPHA * x + mod[:, 2] * y, ln1_g[l], ln1_b[l])
        if not last:
            yc = jnp.concatenate([yc_da, yc_mla, yc_ret], -1) @ w_out[l]
            ctx = layer_norm(ALPHA * ctx + modc[2] * yc, ln1_g[l], ln1_b[l])
        h = x * (1 + mod[:, 4]) + mod[:, 3]
        tokens = h.reshape(B * S, D)
        if not last:
            hc = ctx * (1 + modc[4]) + modc[3]
            tokens = jnp.concatenate([tokens, hc.reshape(B * C, D)], 0)
        f = moe(tokens, router_w, router_b, exp_w1[l], exp_w3[l], exp_w2[l])
        x = layer_norm(ALPHA * x + mod[:, 5] * f[:B * S].reshape(B, S, D), ln2_g[l], ln2_b[l])
        if not last:
            ctx = layer_norm(ALPHA * ctx + modc[5] * f[B * S:].reshape(B, C, D), ln2_g[l], ln2_b[l])
    return x
```

```python
import math
from contextlib import ExitStack
import numpy as np
import ml_dtypes
import concourse.bass as bass
import concourse.mybir as mybir
from concourse.bass_utils import run_bass_kernel_spmd

F32 = mybir.dt.float32
BF16 = mybir.dt.bfloat16
AF = mybir.ActivationFunctionType
ALU = mybir.AluOpType
AX = mybir.AxisListType
NPBF = ml_dtypes.bfloat16

D = 1024
S = 16384
T = 4096
CT = 256
NCORE = 8
DEPTH = 4
NE = 16
ALPHA = (2 * DEPTH) ** 0.25
LN_EPS = 1e-5
RMS_EPS = 1e-6
ROPE_BASE = 10000.0
EPOCH = 30000
CCDMA = 'cc'
DEBUG = False


class Res:
    __slots__ = ("name", "w", "r")

    def __init__(self, name=""):
        self.name = name
        self.w = None
        self.r = {}


class _Rec:
    def __init__(self):
        self.call = None

    def __getattr__(self, name):
        def f(*a, **k):
            self.call = (name, a, k)
            return self
        return f


class Prog:
    STREAMS = ("sp", "pe", "act", "dve", "pool")
    NSLOT = 8

    def __init__(self, nc):
        self.nc = nc
        self.ops = {e: [] for e in self.STREAMS}
        self.cnt = {e: 0 for e in self.STREAMS}
        self.known = {e: {} for e in self.STREAMS}
        self.semkeys = []
        self.semset = set()
        self.dma_slot = {q: 0 for q in self.STREAMS}
        self.dma_val = {}
        self.nops = 0
        self.pending = {e: None for e in self.STREAMS}
        self.sems = {}
        self.es = ExitStack()

    def _sem(self, key):
        if key not in self.semset:
            self.semset.add(key)
            self.semkeys.append(key)
        return key

    def op(self, eng, fn, reads=(), writes=(), dma=None):
        deps = {}

        def add(t):
            if t is None:
                return
            k, v = t
            if deps.get(k, -1) < v:
                deps[k] = v
        for r in reads:
            add(r.w)
        for w in writes:
            add(w.w)
            for t in w.r.items():
                add(t)
        if self.pending[eng]:
            for k, v in self.pending[eng].items():
                if deps.get(k, -1) < v:
                    deps[k] = v
            self.pending[eng] = None
        waits = []
        kn = self.known[eng]
        is_dma = (eng == "sp") if dma is None else dma
        if is_dma == "cc":
            key = self._sem(("cc", eng))
            val = self.dma_val.get(("cc", eng), 0) + 1
            self.dma_val[("cc", eng)] = val
            ticket = (key, val)
            inc = 1
        elif is_dma:
            slot = self.dma_slot[eng]
            self.dma_slot[eng] = (slot + 1) % self.NSLOT
            key = self._sem(("dma", eng, slot))
            prev = self.dma_val.get((eng, slot), 0)
            if prev:
                deps[key] = max(deps.get(key, 0), prev)
            val = prev + 16
            self.dma_val[(eng, slot)] = val
            ticket = (key, val)
            inc = 16
        else:
            n = self.cnt[eng]
            self.cnt[eng] = n + 1
            key = self._sem((eng, n // EPOCH))
            ticket = (key, n % EPOCH + 1)
            inc = 1
        for k, v in deps.items():
            if eng == "pe" and k[0] == "pe":
                continue
            if kn.get(k, 0) >= v:
                continue
            kn[k] = v
            waits.append((k, v))
        rec = _Rec()
        fn(rec)
        self.ops[eng].append((rec.call, waits, key, inc))
        for r in reads:
            if r.r.get(ticket[0], -1) < ticket[1]:
                r.r[ticket[0]] = ticket[1]
        for w in writes:
            w.w = ticket
            w.r = {}
        self.nops += 1
        return ticket

    def barrier(self):
        allk = {}
        for e in self.STREAMS:
            n = self.cnt[e]
            if n:
                allk[(e, (n - 1) // EPOCH)] = (n - 1) % EPOCH + 1
        for (q, slot), v in self.dma_val.items():
            allk[("cc", slot) if q == "cc" else ("dma", q, slot)] = v
        for e in self.STREAMS:
            self.pending[e] = dict(allk)

    def flush(self, final_tickets=()):
        nc = self.nc
        for k in self.semkeys:
            if k not in self.sems:
                self.sems[k] = self.es.enter_context(nc.semaphore("s%d" % len(self.sems)))
        sems = self.sems
        ops = self.ops
        self.ops = {e: [] for e in self.STREAMS}
        with nc.Block() as block:
            def replay(eh, oplist, tail=()):
                for (name, a, kw), waits, key, inc in oplist:
                    for k, v in waits:
                        eh.wait_ge(sems[k], v)
                    getattr(eh, name)(*a, **kw).then_inc(sems[key], inc)
                for k, v in tail:
                    eh.wait_ge(sems[k], v)

            @block.sync
            def _(e):
                replay(e, ops["sp"], tail=final_tickets)

            @block.tensor
            def _(e):
                replay(e, ops["pe"])

            @block.scalar
            def _(e):
                replay(e, ops["act"])

            @block.vector
            def _(e):
                replay(e, ops["dve"])

            @block.gpsimd
            def _(e):
                replay(e, ops["pool"])


def dram_specs():
    sp = {}
    b16, f32 = "bf16", "f32"
    TTL = T + CT
    sp["xT"] = ([8, 128, T], f32)
    sp["cxT"] = ([8, 128, CT], f32)
    for nm in ("qda", "qdaf", "kda_own"):
        sp[nm] = ([384, TTL], b16)
    sp["vda_own"] = ([6 * TTL, 65], b16)
    for nm in ("qml", "qmlf"):
        sp[nm] = ([6, 96, TTL], b16)
    sp["kml_own"] = ([576, TTL], b16)
    sp["vml_own"] = ([6 * TTL, 65], b16)
    for nm in ("qr", "kr", "sg"):
        sp[nm] = ([4, 64, TTL], b16)
    sp["krt"] = ([TTL, 256], b16)
    sp["vr"] = ([TTL, 256], b16)
    sp["rsum_own"] = ([64, 512], f32)
    sp["kda_all"] = ([12 * 4 * 32, TTL], b16)
    sp["vda_all"] = ([6 * 4 * TTL, 65], b16)
    sp["kml_all"] = ([12 * 4 * 48, TTL], b16)
    sp["vml_all"] = ([4 * 6 * TTL, 65], b16)
    sp["rsum_all"] = ([4 * 64, 512], f32)
    for l_ in range(1, DEPTH):
        sp["xTL%d" % l_] = ([8, 128, T], f32)
        sp["cxTL%d" % l_] = ([8, 128, CT], f32)
    sp["xT_out"] = ([8, 128, T], f32)
    sp["yT"] = ([16, 64, TTL], b16)
    sp["xT_next"] = ([8, 128, T], f32)
    sp["cxT_next"] = ([8, 128, CT], f32)
    sp["x1T"] = ([8, 128, TTL], f32)
    return sp


WEIGHT_SPECS = {
    "cvec": [128, 8, 2], "w_ada": [D, 6 * D], "b_adaT": [128, 48], "w_in": [D, 2592],
    "da_lambda": [128], "da_sublnT": [64, 1], "gqT": [128, 2], "w_uq": [256, 576], "gkvT": [128, 1],
    "w_ukv": [128, 768], "decay": [8], "w_out": [D, D], "ln1_gT": [128, 8], "ln1_bT": [128, 8],
    "ln2_gT": [128, 8], "ln2_bT": [128, 8], "router_w": [D, NE], "router_b": [NE],
    "exp_w1": [NE, D, 512], "exp_w3": [NE, D, 512], "exp_w2": [NE, 512, D],
    "ropeA": [2, 128, T], "ropeM": [2, 96, T], "ropeR": [2, 64, T], "tpos": [128, 32],
    "ebound": [128, 16], "ident": [128, 128], "maskpos": [128, 128],
}

TT = T + CT
GROUPS = [(g * 512, 512) for g in range(8)] + [(T, CT)]


class Ctx:
    def __init__(self, ext_in, ext_out):
        self.nc = bass.Bass("TRN2", target_bir_lowering=False)
        self.P = Prog(self.nc)
        self.dr = {}
        self.res = {}
        self.ext_in = ext_in
        self.ext_out = ext_out
        self.out_tickets = []

    def dram(self, name, shape=None, dt=None):
        if name in self.dr:
            return self.dr[name]
        if name in self.ext_in:
            kind = "ExternalInput"
        elif name in self.ext_out:
            kind = "ExternalOutput"
        else:
            kind = "Internal"
        t = self.nc.dram_tensor(name, list(shape), dt, kind=kind).ap()
        self.dr[name] = t
        self.res[name] = Res(name)
        return t


def build_program(stages, layer_of, ext_in, ext_out, wshapes):
    cx = Ctx(ext_in, ext_out)
    nc, P = cx.nc, cx.P
    specs = dram_specs()
    DT = {"bf16": BF16, "f32": F32}

    def dget(name):
        shp, dt = specs[name]
        return cx.dram(name, shp, DT[dt])

    def xnames(l):
        if l == 0:
            return "xT", "cxT"
        if l == DEPTH:
            return "xT_out", None
        return "xTL%d" % l, "cxTL%d" % l

    class WL:
        def __init__(self, ap):
            self.ap = ap

        def __getitem__(self, key):
            if isinstance(key, tuple):
                return self.ap[(layer_of[key[0]],) + tuple(key[1:])]
            return self.ap[layer_of[key]]
    W = {}
    PERLAYER = ("w_ada", "b_adaT", "w_in", "da_lambda", "da_sublnT", "gqT", "w_uq", "gkvT", "w_ukv", "decay", "w_out",
                "ln1_gT", "ln1_bT", "ln2_gT", "ln2_bT", "exp_w1", "exp_w3", "exp_w2")
    for nm, shp in wshapes.items():
        t = nc.dram_tensor(nm, list(shp), F32, kind="ExternalInput").ap()
        W[nm] = WL(t) if nm in PERLAYER else t

    es_glob = ExitStack()
    ps_all = es_glob.enter_context(nc.psum_tensor("ps_all", [128, 4096], F32))
    psb = [ps_all[:, i * 512:(i + 1) * 512] for i in range(8)]
    psbf = ps_all[:, 3584:4096].bitcast(BF16)
    psr = [Res("ps%d" % i) for i in range(8)]
    psbf_r = psr[7]
    rot = {"i": 0}

    def next_ps(lo=0, hi=6):
        i = lo + rot["i"] % (hi - lo)
        rot["i"] += 1
        return psb[i], psr[i]

    def dma(q, out, in_, reads=(), writes=()):
        return P.op(q, lambda e: e.dma_start(out=out, in_=in_), reads, writes, dma=True)

    ucnt = {"n": 0}

    def sbuf(es, name, shape, dt):
        ucnt["n"] += 1
        return es.enter_context(nc.sbuf_tensor("sb%d_%s" % (ucnt["n"], name), list(shape), dt))

    ident = sbuf(es_glob, "ident", [128, 128], F32)
    identb = sbuf(es_glob, "identb", [128, 128], BF16)
    ones = sbuf(es_glob, "ones", [128, 128], F32)
    epsr = sbuf(es_glob, "epsr", [128, 2], F32)
    r_const = Res("const")
    dma("sp", ident[:], W["ident"][:, :], writes=[r_const])
    P.op("dve", lambda e: e.tensor_copy(out=identb[:], in_=ident[:]), reads=[r_const], writes=[r_const])
    P.op("dve", lambda e: e.memset(ones[:], 1.0), writes=[r_const])
    P.op("dve", lambda e: e.memset(epsr[:, 0:1], RMS_EPS), writes=[r_const])
    P.op("dve", lambda e: e.memset(epsr[:, 1:2], LN_EPS), writes=[r_const])

    def compute_mod(es, l):
        modT = sbuf(es, "modT%d" % l, [128, 48, 2], F32)
        r_mod = Res("mod")
        with ExitStack() as e2:
            sc = sbuf(e2, "sc", [128, 8, 2], F32)
            bad = sbuf(e2, "bad", [128, 48], F32)
            stg = [sbuf(e2, "stgA%d" % i, [128, 8, 512], F32) for i in range(2)]
            r_sc, r_bad = Res(), Res()
            r_stg = [Res(), Res()]
            dma("sp", sc[:], W["cvec"][:, :, :], writes=[r_sc])
            dma("sp", bad[:], W["b_adaT"][l], writes=[r_bad])
            P.op("act", lambda e: e.activation(out=sc[:], in_=sc[:], func=AF.Silu), reads=[r_sc], writes=[r_sc])
            pm, pmr = psb[6], psr[6]
            wv = W["w_ada"][l].rearrange("(k p) n -> p k n", p=128)
            for jb in range(12):
                st, rs = stg[jb % 2], r_stg[jb % 2]
                dma("sp", st[:], wv[:, :, jb * 512:(jb + 1) * 512], writes=[rs])
                for j in range(4):
                    col = (jb * 4 + j) * 2
                    for k in range(8):
                        P.op("pe", lambda e, st=st, j=j, k=k, col=col: e.matmul(
                            pm[:, col:col + 2], lhsT=st[:, k, j * 128:(j + 1) * 128], rhs=sc[:, k, :],
                            start=(k == 0), stop=(k == 7)), reads=[rs, r_sc], writes=[pmr])
            for i in range(2):
                P.op("dve", lambda e, i=i: e.tensor_tensor(out=modT[:, :, i], in0=pm[:, i:96:2], in1=bad[:],
                                                          op=ALU.add), reads=[pmr, r_bad], writes=[r_mod])
            for m in (1, 4):
                P.op("dve", lambda e, m=m: e.tensor_scalar_add(out=modT[:, m * 8:(m + 1) * 8, :],
                                                              in0=modT[:, m * 8:(m + 1) * 8, :], scalar1=1.0),
                     reads=[r_mod], writes=[r_mod])
            P.barrier()
        return modT, r_mod

    def load_lg(es, l):
        lg = sbuf(es, "lg", [128, 8], F32)
        r_lg = Res("lg")
        dma("sp", lg[:], W["decay"][l].partition_broadcast(128), writes=[r_lg])
        P.op("act", lambda e: e.activation(out=lg[:], in_=lg[:], func=AF.Exp, scale=-1.0), reads=[r_lg], writes=[r_lg])
        P.op("act", lambda e: e.activation(out=lg[:], in_=lg[:], func=AF.Ln, bias=ones[:, 0:1], scale=1.0),
             reads=[r_lg, r_const], writes=[r_lg])
        P.op("dve", lambda e: e.tensor_scalar_mul(out=lg[:], in0=lg[:], scalar1=-1.0), reads=[r_lg], writes=[r_lg])
        return lg, r_lg

    def stage_A(l):
        es = ExitStack()
        modT, r_mod = compute_mod(es, l)
        lg, r_lg = load_lg(es, l)
        xn, cxn = xnames(l)
        xT, cxT = dget(xn), dget(cxn)
        win = sbuf(es, "win", [128, 8, 2592], BF16)
        wsw = sbuf(es, "wsw", [128, 8, 1312], BF16)
        wuq = sbuf(es, "wuq", [128, 2, 576], BF16)
        wuqs = sbuf(es, "wuqs", [128, 2, 576], BF16)
        wukv = sbuf(es, "wukv", [128, 768], BF16)
        gq = sbuf(es, "gq", [128, 3], F32)
        r_w = Res("w")
        dma("sp", gq[:, 0:2], W["gqT"][l], writes=[r_w])
        dma("sp", gq[:, 2:3], W["gkvT"][l], writes=[r_w])
        with ExitStack() as e2:
            stg = [sbuf(e2, "stgw%d" % i, [128, 2592], F32) for i in range(2)]
            r_stg = [Res(), Res()]
            for k in range(8):
                st, rs = stg[k % 2], r_stg[k % 2]
                dma("sp", st[:], W["w_in"][l, k * 128:(k + 1) * 128, :], writes=[rs])
                P.op("dve", lambda e, st=st: e.tensor_scalar_mul(out=st[:, 1824:2080], in0=st[:, 1824:2080], scalar1=0.125),
                     reads=[rs], writes=[rs])
                P.op("dve", lambda e, st=st, k=k: e.tensor_copy(out=win[:, k, :], in_=st[:]), reads=[rs], writes=[r_w])
                for (s0, n, d0) in ((0, 768, 0), (1536, 544, 768)):
                    P.op("pool", lambda e, st=st, k=k, s0=s0, n=n, d0=d0: e.tensor_scalar_mul(
                        out=wsw[:, k, d0:d0 + n:2], in0=st[:, s0 + 1:s0 + n:2], scalar1=-1.0), reads=[rs], writes=[r_w])
                    P.op("pool", lambda e, st=st, k=k, s0=s0, n=n, d0=d0: e.tensor_copy(
                        out=wsw[:, k, d0 + 1:d0 + n:2], in_=st[:, s0:s0 + n:2]), reads=[rs], writes=[r_w])
            st = stg[0]
            stv = st[:, 0:1152].rearrange("p (k n) -> p k n", k=2)
            dma("sp", stv, W["w_uq"][l].rearrange("(k p) n -> p k n", p=128), writes=[r_stg[0]])
            P.op("pool", lambda e: e.memset(wuqs[:], 0.0), writes=[r_w])
            for k in range(2):
                P.op("dve", lambda e, k=k: e.tensor_scalar_mul(out=stv[:, k, :], in0=stv[:, k, :], scalar1=gq[:, k:k + 1]),
                     reads=[r_stg[0], r_w], writes=[r_stg[0]])
                P.op("dve", lambda e, k=k: e.tensor_copy(out=wuq[:, k, :], in_=stv[:, k, :]), reads=[r_stg[0]], writes=[r_w])
                sv = stv[:, k, :].rearrange("p (h d) -> p h d", h=6)
                dv = wuqs[:, k, :].rearrange("p (h d) -> p h d", h=6)
                P.op("pool", lambda e, sv=sv, dv=dv: e.tensor_scalar_mul(out=dv[:, :, 64:96:2], in0=sv[:, :, 65:96:2], scalar1=-1.0),
                     reads=[r_stg[0]], writes=[r_w])
                P.op("pool", lambda e, sv=sv, dv=dv: e.tensor_copy(out=dv[:, :, 65:96:2], in_=sv[:, :, 64:96:2]),
                     reads=[r_stg[0]], writes=[r_w])
            st1 = stg[1]
            dma("sp", st1[:, 0:768], W["w_ukv"][l], writes=[r_stg[1]])
            P.op("dve", lambda e: e.tensor_scalar_mul(out=wukv[:], in0=st1[:, 0:768], scalar1=gq[:, 2:3]),
                 reads=[r_stg[1], r_w], writes=[r_w])
            P.barrier()

        tpos = sbuf(es, "tpos", [128, 32], F32)
        epos = sbuf(es, "epos", [128, 32], F32)
        wfb = sbuf(es, "wfb", [128, 32, 8], F32)
        r_wfb = Res("wfb")
        dma("sp", tpos[:], W["tpos"][:, :], writes=[r_wfb])
        P.op("dve", lambda e: e.tensor_scalar(out=epos[:], in0=tpos[:], scalar1=-1.0, scalar2=float(T - 1),
                                              op0=ALU.mult, op1=ALU.add), reads=[r_wfb], writes=[r_wfb])
        for h in range(4):
            P.op("act", lambda e, h=h: e.activation(out=wfb[:, :, h], in_=epos[:], func=AF.Exp, scale=lg[:, h:h + 1]),
                 reads=[r_wfb, r_lg], writes=[r_wfb])
            P.op("act", lambda e, h=h: e.activation(out=wfb[:, :, 4 + h], in_=tpos[:], func=AF.Exp, scale=lg[:, 4 + h:5 + h]),
                 reads=[r_wfb, r_lg], writes=[r_wfb])

        NB = 2
        xg = [sbuf(es, "xg%d" % i, [128, 8, 512], F32) for i in range(NB)]
        hT = [sbuf(es, "hT%d" % i, [128, 8, 512], BF16) for i in range(NB)]
        r_xg = [Res() for _ in range(NB)]
        r_hT = [Res() for _ in range(NB)]
        tabA = [sbuf(es, "tabA%d" % i, [128, 2, 512], F32) for i in range(NB)]
        tabM = [sbuf(es, "tabM%d" % i, [96, 2, 512], F32) for i in range(NB)]
        tabR = [sbuf(es, "tabR%d" % i, [64, 2, 512], F32) for i in range(NB)]
        r_tab = [Res() for _ in range(NB)]
        NTMP = 3
        tmp1 = [sbuf(es, "tmp1_%d" % i, [128, 512], F32) for i in range(NTMP)]
        tmp2 = [sbuf(es, "tmp2_%d" % i, [128, 512], F32) for i in range(NTMP)]
        r_tmp = [Res() for _ in range(NTMP)]
        NOB = 4
        ob = [sbuf(es, "ob%d" % i, [128, 512], BF16) for i in range(NOB)]
        ob2 = [sbuf(es, "ob2_%d" % i, [128, 512], BF16) for i in range(NOB)]
        r_ob = [Res() for _ in range(NOB)]
        r_ob2 = [Res() for _ in range(NOB)]
        cqn = sbuf(es, "cqn", [128, 2, 512], BF16)
        ckvn = sbuf(es, "ckvn", [128, 512], BF16)
        sq = sbuf(es, "sq", [128, 3, 512], F32)
        rstd = sbuf(es, "rstd", [128, 2, 512], F32)
        r_cqn, r_ckvn, r_sq, r_rstd = Res(), Res(), Res(), Res()
        vt = [sbuf(es, "vt%d" % i, [128, 6, 65], BF16) for i in range(4)]
        r_vt = [Res() for _ in range(4)]
        for i in range(4):
            P.op("pool", lambda e, i=i: e.memset(vt[i][:], 1.0), writes=[r_vt[i]])
        vrt = [sbuf(es, "vrt%d" % i, [128, 256], BF16) for i in range(2)]
        krt_t = [sbuf(es, "krt_t%d" % i, [128, 256], BF16) for i in range(2)]
        kw = [sbuf(es, "kw%d" % i, [128, 8, 64], BF16) for i in range(2)]
        r_vrt = [Res(), Res()]
        r_krt = [Res(), Res()]
        r_kw = [Res(), Res()]
        krg = sbuf(es, "krg", [64, 4, 512], BF16)
        r_krg = Res()
        cnt = {"tmp": 0, "ob": 0, "vt": 0}
        psU, rU = psb[6], psr[6]

        D_ = {n: dget(n) for n in ("qda", "qdaf", "kda_own", "vda_own", "qml", "qmlf", "kml_own", "vml_own",
                                    "qr", "kr", "krt", "vr", "sg", "rsum_own")}
        rD = cx.res

        def proj(M, N, terms, reads):
            ps, pr = next_ps()
            n = len(terms)
            for i, (lh, rh) in enumerate(terms):
                P.op("pe", lambda e, lh=lh, rh=rh, i=i: e.matmul(ps[0:M, 0:N], lhsT=lh, rhs=rh, start=(i == 0), stop=(i == n - 1)),
                     reads=reads, writes=[pr])
            return ps, pr

        def rope(M, N, pa, ra, pb, rb, tab, rt, rows0=0):
            i = cnt["tmp"] % NTMP
            cnt["tmp"] += 1
            j = cnt["ob"] % NOB
            cnt["ob"] += 1
            P.op("dve", lambda e: e.tensor_tensor(out=tmp1[i][0:M, 0:N], in0=pa[0:M, 0:N], in1=tab[rows0:rows0 + M, 0, 0:N], op=ALU.mult),
                 reads=[ra, rt], writes=[r_tmp[i]])
            P.op("dve", lambda e: e.tensor_tensor(out=tmp2[i][0:M, 0:N], in0=pb[0:M, 0:N], in1=tab[rows0:rows0 + M, 1, 0:N], op=ALU.mult),
                 reads=[rb, rt], writes=[r_tmp[i]])
            P.op("pool", lambda e: e.tensor_tensor(out=ob[j][0:M, 0:N], in0=tmp1[i][0:M, 0:N], in1=tmp2[i][0:M, 0:N], op=ALU.add),
                 reads=[r_tmp[i]], writes=[r_ob[j]])
            return ob[j], r_ob[j]

        def plain(M, N, pa, ra, func=AF.Copy):
            j = cnt["ob"] % NOB
            cnt["ob"] += 1
            P.op("act", lambda e: e.activation(out=ob2[j][0:M, 0:N], in_=pa[0:M, 0:N], func=func), reads=[ra], writes=[r_ob2[j]])
            return ob2[j], r_ob2[j]

        for gi, (t0, N) in enumerate(GROUPS):
            b = gi % NB
            isctx = (t0 == T)
            mi = 1 if isctx else 0
            src = cxT.rearrange("c p t -> p c t") if isctx else xT.rearrange("c p t -> p c t")[:, :, t0:t0 + N]
            dma("sp", xg[b][:, :, 0:N], src, reads=[rD[cxn if isctx else xn]], writes=[r_xg[b]])
            dma("sp", tabA[b][:, :, 0:N], W["ropeA"].rearrange("c p t -> p c t")[:, :, t0:t0 + N], writes=[r_tab[b]])
            dma("sp", tabM[b][:, :, 0:N], W["ropeM"].rearrange("c p t -> p c t")[:, :, t0:t0 + N], writes=[r_tab[b]])
            dma("sp", tabR[b][:, :, 0:N], W["ropeR"].rearrange("c p t -> p c t")[:, :, t0:t0 + N], writes=[r_tab[b]])
            for c in range(8):
                P.op("dve", lambda e, c=c: e.tensor_scalar(out=hT[b][:, c, 0:N], in0=xg[b][:, c, 0:N],
                                                          scalar1=modT[:, 8 + c, mi:mi + 1], scalar2=modT[:, c, mi:mi + 1],
                                                          op0=ALU.mult, op1=ALU.add), reads=[r_xg[b], r_mod], writes=[r_hT[b]])
            hb, rh = hT[b], r_hT[b]
            if gi == 0 and DEBUG:
                d1 = nc.dram_tensor("dbg_mod", [128, 96], F32, kind="ExternalOutput").ap()
                d2 = nc.dram_tensor("dbg_h", [128, 8, 512], BF16, kind="ExternalOutput").ap()
                d3 = nc.dram_tensor("dbg_win", [128, 8, 2592], BF16, kind="ExternalOutput").ap()
                dma("sp", d1[:, :], modT[:].rearrange("p a b -> p (a b)"), reads=[r_mod])
                dma("sp", d2[:, :, :], hb[:], reads=[rh])
                dma("sp", d3[:, :, :], win[:], reads=[r_w])

            def wterms(wt, c0, M):
                return [(wt[:, k, c0:c0 + M], hb[:, k, 0:N]) for k in range(8)]

            for c in range(3):
                pa, ra = proj(128, N, wterms(win, c * 128, 128), [rh, r_w])
                pb, rb = proj(128, N, wterms(wsw, c * 128, 128), [rh, r_w])
                o, ro = rope(128, N, pa, ra, pb, rb, tabA[b], r_tab[b])
                dma("act", D_["qda"][c * 128:(c + 1) * 128, t0:t0 + N], o[:, 0:N], reads=[ro], writes=[rD["qda"]])
                o2, ro2 = plain(128, N, pa, ra)
                dma("act", D_["qdaf"][c * 128:(c + 1) * 128, t0:t0 + N], o2[:, 0:N], reads=[ro2], writes=[rD["qdaf"]])
            for c in range(3):
                pa, ra = proj(128, N, wterms(win, 384 + c * 128, 128), [rh, r_w])
                pb, rb = proj(128, N, wterms(wsw, 384 + c * 128, 128), [rh, r_w])
                o, ro = rope(128, N, pa, ra, pb, rb, tabA[b], r_tab[b])
                dma("act", D_["kda_own"][c * 128:(c + 1) * 128, t0:t0 + N], o[:, 0:N], reads=[ro], writes=[rD["kda_own"]])
            pcq = [proj(128, N, wterms(win, 1152 + c * 128, 128), [rh, r_w]) for c in range(2)]
            pckv = proj(128, N, wterms(win, 1408, 128), [rh, r_w])
            for c, (pp, rr) in enumerate(pcq + [pckv]):
                P.op("act", lambda e, c=c, pp=pp: e.activation(out=sq[:, c, 0:N], in_=pp[:, 0:N], func=AF.Square), reads=[rr], writes=[r_sq])
            pbq, rbq = proj(128, N, [(ones[:, :], sq[:, c, 0:N]) for c in range(2)], [r_sq, r_const])
            pbk, rbk = proj(128, N, [(ones[:, :], sq[:, 2, 0:N])], [r_sq, r_const])
            P.op("act", lambda e: e.activation(out=rstd[:, 0, 0:N], in_=pbq[:, 0:N], func=AF.Sqrt, scale=1.0 / 256, bias=epsr[:, 0:1]),
                 reads=[rbq, r_const], writes=[r_rstd])
            P.op("act", lambda e: e.activation(out=rstd[:, 1, 0:N], in_=pbk[:, 0:N], func=AF.Sqrt, scale=1.0 / 128, bias=epsr[:, 0:1]),
                 reads=[rbk, r_const], writes=[r_rstd])
            P.op("dve", lambda e: e.reciprocal(out=rstd[:, :, 0:N], in_=rstd[:, :, 0:N]), reads=[r_rstd], writes=[r_rstd])
            for c in range(2):
                P.op("dve", lambda e, c=c: e.tensor_tensor(out=cqn[:, c, 0:N], in0=pcq[c][0][:, 0:N], in1=rstd[:, 0, 0:N], op=ALU.mult),
                     reads=[pcq[c][1], r_rstd], writes=[r_cqn])
            P.op("dve", lambda e: e.tensor_tensor(out=ckvn[:, 0:N], in0=pckv[0][:, 0:N], in1=rstd[:, 1, 0:N], op=ALU.mult),
                 reads=[pckv[1], r_rstd], writes=[r_ckvn])
            for h in range(6):
                pa, ra = proj(96, N, [(wuq[:, k, h * 96:(h + 1) * 96], cqn[:, k, 0:N]) for k in range(2)], [r_cqn, r_w])
                pb, rb = proj(96, N, [(wuqs[:, k, h * 96:(h + 1) * 96], cqn[:, k, 0:N]) for k in range(2)], [r_cqn, r_w])
                o, ro = rope(96, N, pa, ra, pb, rb, tabM[b], r_tab[b])
                dma("act", D_["qml"][h, :, t0:t0 + N], o[0:96, 0:N], reads=[ro], writes=[rD["qml"]])
                o2, ro2 = plain(96, N, pa, ra)
                dma("act", D_["qmlf"][h, :, t0:t0 + N], o2[0:96, 0:N], reads=[ro2], writes=[rD["qmlf"]])
            for c in range(3):
                pa, ra = proj(128, N, [(wukv[:, c * 128:(c + 1) * 128], ckvn[:, 0:N])], [r_ckvn, r_w])
                o2, ro2 = plain(128, N, pa, ra)
                for hh in range(2):
                    dma("act", D_["kml_own"][(2 * c + hh) * 96:(2 * c + hh) * 96 + 64, t0:t0 + N], o2[hh * 64:(hh + 1) * 64, 0:N], reads=[ro2],
                        writes=[rD["kml_own"]])
            pa, ra = proj(32, N, wterms(win, 1536, 32), [rh, r_w])
            pb, rb = proj(32, N, wterms(wsw, 768, 32), [rh, r_w])
            o, ro = rope(32, N, pa, ra, pb, rb, tabA[b], r_tab[b])
            for h in range(6):
                dma("act", D_["kml_own"][h * 96 + 64:h * 96 + 96, t0:t0 + N], o[0:32, 0:N], reads=[ro], writes=[rD["kml_own"]])
            for h in range(4):
                pa, ra = proj(64, N, wterms(win, 1568 + h * 64, 64), [rh, r_w])
                pb, rb = proj(64, N, wterms(wsw, 800 + h * 64, 64), [rh, r_w])
                o, ro = rope(64, N, pa, ra, pb, rb, tabR[b], r_tab[b])
                dma("act", D_["qr"][h, :, t0:t0 + N], o[0:64, 0:N], reads=[ro], writes=[rD["qr"]])
            for h in range(4):
                pa, ra = proj(64, N, wterms(win, 1824 + h * 64, 64), [rh, r_w])
                pb, rb = proj(64, N, wterms(wsw, 1056 + h * 64, 64), [rh, r_w])
                o, ro = rope(64, N, pa, ra, pb, rb, tabR[b], r_tab[b])
                dma("act", D_["kr"][h, :, t0:t0 + N], o[0:64, 0:N], reads=[ro], writes=[rD["kr"]])
                P.op("pool", lambda e, h=h, o=o: e.tensor_copy(out=krg[:, h, 0:N], in_=o[0:64, 0:N]), reads=[ro], writes=[r_krg])
            for h in range(4):
                pa, ra = proj(64, N, wterms(win, 2336 + h * 64, 64), [rh, r_w])
                o2, ro2 = plain(64, N, pa, ra, func=AF.Silu)
                dma("act", D_["sg"][h, :, t0:t0 + N], o2[0:64, 0:N], reads=[ro2], writes=[rD["sg"]])
            for j in range(N // 128):
                ts = slice(j * 128, (j + 1) * 128)
                tg = t0 + j * 128
                for (wt_terms, dname) in (([(hb[:, k, ts], win[:, k, 768:1152]) for k in range(8)], "vda_own"),
                                          ([(ckvn[:, ts], wukv[:, 384:768])], "vml_own")):
                    ps, pr = next_ps()
                    n = len(wt_terms)
                    for i, (lh, rhh) in enumerate(wt_terms):
                        P.op("pe", lambda e, ps=ps, lh=lh, rhh=rhh, i=i, n=n: e.matmul(ps[:, 0:384], lhsT=lh, rhs=rhh, start=(i == 0), stop=(i == n - 1)),
                             reads=[rh, r_w, r_ckvn], writes=[pr])
                    vi = cnt["vt"] % 4
                    cnt["vt"] += 1
                    P.op("dve", lambda e, ps=ps, vi=vi: e.tensor_copy(out=vt[vi][:, :, 0:64], in_=ps[:, 0:384].rearrange("p (h d) -> p h d", h=6)),
                         reads=[pr], writes=[r_vt[vi]])
                    dma("act", D_[dname].rearrange("(h t) e -> t h e", h=6)[tg:tg + 128, :, :], vt[vi][:], reads=[r_vt[vi]], writes=[rD[dname]])
                jj = j % 2
                ps, pr = next_ps()
                for k in range(8):
                    P.op("pe", lambda e, ps=ps, k=k: e.matmul(ps[:, 0:256], lhsT=hb[:, k, ts], rhs=win[:, k, 2080:2336], start=(k == 0), stop=(k == 7)),
                         reads=[rh, r_w], writes=[pr])
                P.op("dve", lambda e, ps=ps, jj=jj: e.tensor_copy(out=vrt[jj][:], in_=ps[:, 0:256]), reads=[pr], writes=[r_vrt[jj]])
                dma("act", D_["vr"][tg:tg + 128, :], vrt[jj][:], reads=[r_vrt[jj]], writes=[rD["vr"]])
                for h in range(4):
                    P.op("pe", lambda e, h=h: e.transpose(psbf[:, h * 64:(h + 1) * 64], krg[:, h, ts], identb[0:64, 0:64]),
                         reads=[r_krg, r_const], writes=[psbf_r])
                P.op("dve", lambda e, jj=jj: e.tensor_copy(out=krt_t[jj][:], in_=psbf[:, 0:256]), reads=[psbf_r], writes=[r_krt[jj]])
                dma("act", D_["krt"][tg:tg + 128, :], krt_t[jj][:], reads=[r_krt[jj]], writes=[rD["krt"]])
                if not isctx:
                    tile_i = tg // 128
                    for dd in range(2):
                        for h in range(4):
                            P.op("pool", lambda e, jj=jj, dd=dd, h=h, tile_i=tile_i: e.tensor_scalar_mul(
                                out=kw[jj][:, dd * 4 + h, :], in0=krt_t[jj][:, h * 64:(h + 1) * 64],
                                scalar1=wfb[:, tile_i, dd * 4 + h:dd * 4 + h + 1]), reads=[r_krt[jj], r_wfb], writes=[r_kw[jj]])
                    for dd in range(2):
                        for h in range(4):
                            col = (dd * 4 + h) * 64
                            P.op("pe", lambda e, jj=jj, dd=dd, h=h, col=col, tile_i=tile_i: e.matmul(
                                psU[0:64, col:col + 64], lhsT=kw[jj][:, dd * 4 + h, :], rhs=vrt[jj][:, h * 64:(h + 1) * 64],
                                start=(tile_i == 0 and dd == 0 and h == 0), stop=(tile_i == 31 and dd == 1 and h == 3)), reads=[r_kw[jj], r_vrt[jj]], writes=[rU])
        usb = sbuf(es, "usb", [64, 512], F32)
        r_usb = Res()
        P.op("dve", lambda e: e.tensor_copy(out=usb[:], in_=psU[0:64, :]), reads=[rU], writes=[r_usb])
        dma("act", D_["rsum_own"][:, :], usb[:], reads=[r_usb], writes=[rD["rsum_own"]])
        P.barrier()
        P.flush()
        es.close()

    def stage_X(l):
        RG = [[0, 1, 2, 3], [4, 5, 6, 7]]

        def ag(a, b_, r0, n, o0):
            src, dst = dget(a), dget(b_)
            P.op("pool", lambda e: e.collective_compute("AllGather", ALU.bypass, replica_groups=RG, ins=[src[r0:r0 + n, :]],
                                                        outs=[dst[o0:o0 + 4 * n, :]]),
                 reads=[cx.res[a]], writes=[cx.res[b_]], dma=CCDMA)
        for mi in range(12):
            ag("kda_own", "kda_all", mi * 32, 32, mi * 128)
        for ch in range(12):
            ag("kml_own", "kml_all", ch * 48, 48, ch * 192)
        for h in range(6):
            ag("vda_own", "vda_all", h * TT, TT, h * 4 * TT)
            ag("vml_own", "vml_all", h * TT, TT, h * 4 * TT)
        ag("rsum_own", "rsum_all", 0, 64, 0)
        P.barrier()
        P.flush()

    def stage_B(l, last):
        es = ExitStack()
        yT = dget("yT")
        rY = cx.res["yT"]
        lam = sbuf(es, "lam", [128, 128], F32)
        lsc = sbuf(es, "lsc", [128, 4], F32)
        gsub = sbuf(es, "gsub", [64, 1], F32)
        r_l = Res()
        lam_init = 0.8 - 0.6 * math.exp(-0.3 * l)
        dma("sp", lam[:], W["da_lambda"][l].partition_broadcast(128), writes=[r_l])
        dma("sp", gsub[:], W["da_sublnT"][l], writes=[r_l])
        P.op("dve", lambda e: e.tensor_tensor(out=lam[:, 0:32], in0=lam[:, 0:32], in1=lam[:, 32:64], op=ALU.mult), reads=[r_l], writes=[r_l])
        P.op("dve", lambda e: e.tensor_tensor(out=lam[:, 64:96], in0=lam[:, 64:96], in1=lam[:, 96:128], op=ALU.mult), reads=[r_l], writes=[r_l])
        P.op("dve", lambda e: e.reduce_sum(out=lsc[:, 0:1], in_=lam[:, 0:32], axis=AX.X), reads=[r_l], writes=[r_l])
        P.op("dve", lambda e: e.reduce_sum(out=lsc[:, 1:2], in_=lam[:, 64:96], axis=AX.X), reads=[r_l], writes=[r_l])
        P.op("act", lambda e: e.activation(out=lsc[:, 0:2], in_=lsc[:, 0:2], func=AF.Exp), reads=[r_l], writes=[r_l])
        P.op("dve", lambda e: e.tensor_tensor(out=lsc[:, 2:3], in0=lsc[:, 0:1], in1=lsc[:, 1:2], op=ALU.subtract), reads=[r_l], writes=[r_l])
        P.op("dve", lambda e: e.tensor_scalar(out=lsc[:, 3:4], in0=lsc[:, 2:3], scalar1=lam_init, scalar2=-1.0, op0=ALU.add, op1=ALU.mult),
             reads=[r_l], writes=[r_l])
        P.op("dve", lambda e: e.tensor_scalar_mul(out=gsub[:], in0=gsub[:], scalar1=(1.0 - lam_init)), reads=[r_l], writes=[r_l])
        sel = sbuf(es, "sel", [65, 64], F32)
        P.op("dve", lambda e: e.memset(sel[:], 0.0), writes=[r_l])
        P.op("dve", lambda e: e.memset(sel[64:65, :], 1.0), writes=[r_l])

        KT = [sbuf(es, "KT%d" % i, [128, S + CT], BF16) for i in range(2)]
        VV = [sbuf(es, "VV%d" % i, [128, 130, 65], BF16) for i in range(2)]
        QT = [sbuf(es, "QT%d" % i, [128, 2, TT], BF16) for i in range(2)]
        r_KT = [Res(), Res()]
        r_VV = [Res(), Res()]
        r_QT = [Res(), Res()]
        NPT = 3
        PT = [sbuf(es, "PT%d" % i, [128, 512], BF16) for i in range(NPT)]
        r_PT = [Res() for _ in range(NPT)]
        osb = [sbuf(es, "osb%d" % i, [65, 512], F32) for i in range(2)]
        r_osb = [Res(), Res()]
        rec = [sbuf(es, "rec%d" % i, [64, 512], F32) for i in range(2)]
        r_rec = [Res(), Res()]
        od = sbuf(es, "od", [64, 512], F32)
        od2 = sbuf(es, "od2", [64, 512], F32)
        r_od = Res()
        yb = [sbuf(es, "yb%d" % i, [64, 512], BF16) for i in range(2)]
        r_yb = [Res(), Res()]
        ctr = {"pt": 0, "u": 0, "y": 0}
        kda_all, vda_all, kml_all, vml_all = dget("kda_all"), dget("vda_all"), dget("kml_all"), dget("vml_all")
        kda_own, vda_own, kml_own, vml_own = dget("kda_own"), dget("vda_own"), dget("kml_own"), dget("vml_own")
        qda, qdaf, qml, qmlf = dget("qda"), dget("qdaf"), dget("qml"), dget("qmlf")
        rD = cx.res

        SG = [ps_all[:, 0:1536], ps_all[:, 1536:3072]]
        r_SG = [Res(), Res()]
        PT3 = [sbuf(es, "PT3_%d" % i, [128, 3, 512], BF16) for i in range(2)]
        r_PT3 = [Res(), Res()]

        def attend(kt, rk, vv, rv, qt, rq, dk, scale, t0, N, isctx, ps_o, r_o):
            tiles = ([] if isctx else [(j, False) for j in range(128)]) + [(128, True), (129, True)]
            grp = [tiles[i:i + 3] for i in range(0, len(tiles), 3)]
            G = len(grp)
            nt = len(tiles)

            def qk(g):
                sg, rs = SG[g % 2], r_SG[g % 2]
                for k, (j, cx_) in enumerate(grp[g]):
                    pb = 32 * k if dk == 32 else 0
                    if cx_:
                        lh = kt[pb:pb + dk, S + (j - 128) * 128:S + (j - 127) * 128]
                        rh_ = qt[pb:pb + dk, 1, t0:t0 + N]
                    else:
                        lh = kt[pb:pb + dk, j:S:128]
                        rh_ = qt[pb:pb + dk, 0, t0:t0 + N]
                    P.op("pe", lambda e: e.matmul(sg[:, k * 512:k * 512 + N], lhsT=lh, rhs=rh_, start=True, stop=True), reads=[rk, rq], writes=[rs])

            def ex(g):
                sg, rs = SG[g % 2], r_SG[g % 2]
                ng = len(grp[g])
                P.op("act", lambda e: e.activation(out=PT3[g % 2][:, 0:ng, 0:N], in_=sg.rearrange("p (k n) -> p k n", k=3)[:, 0:ng, 0:N],
                                                   func=AF.Exp, scale=scale), reads=[rs], writes=[r_PT3[g % 2]])

            def pv(g):
                for k, (j, cx_) in enumerate(grp[g]):
                    ti = g * 3 + k
                    P.op("pe", lambda e: e.matmul(ps_o[0:65, 0:N], lhsT=vv[:, j, :], rhs=PT3[g % 2][:, k, 0:N], start=(ti == 0), stop=(ti == nt - 1)),
                         reads=[rv, r_PT3[g % 2]], writes=[r_o])
            qk(0)
            for g in range(G):
                if g + 1 < G:
                    qk(g + 1)
                ex(g)
                pv(g)

        def normalise(ps_o, r_o, N):
            u = ctr["u"] % 2
            ctr["u"] += 1
            P.op("dve", lambda e: e.tensor_copy(out=osb[u][:, 0:N], in_=ps_o[0:65, 0:N]), reads=[r_o], writes=[r_osb[u]])
            P.op("pe", lambda e: e.matmul(ps_o[0:64, 0:N], lhsT=sel[:, :], rhs=osb[u][:, 0:N], start=True, stop=True), reads=[r_osb[u], r_l], writes=[r_o])
            P.op("dve", lambda e: e.reciprocal(out=rec[u][:, 0:N], in_=ps_o[0:64, 0:N]), reads=[r_o], writes=[r_rec[u]])
            P.op("dve", lambda e: e.tensor_tensor(out=rec[u][:, 0:N], in0=rec[u][:, 0:N], in1=osb[u][0:64, 0:N], op=ALU.mult),
                 reads=[r_rec[u], r_osb[u]], writes=[r_rec[u]])
            return rec[u], r_rec[u]

        qgroups = [(t0, N, False) for (t0, N) in GROUPS[:8]] + ([] if last else [(T, CT, True)])
        o1all = sbuf(es, "o1all", [64, TT], F32)
        r_o1all = Res()
        units = [("da", h, m) for h in range(6) for m in range(2)] + [("ml", h, 0) for h in range(6)]
        for ui, (kind, h, m) in enumerate(units):
            bsel = ui % 2
            kt, rk, vv, rv, qt, rq = KT[bsel], r_KT[bsel], VV[bsel], r_VV[bsel], QT[bsel], r_QT[bsel]
            if kind == "da":
                r0 = (h * 2 + m) * 32
                for pg in range(3):
                    for rr in range(4):
                        dma("sp", kt[32 * pg:32 * pg + 32, rr * T:(rr + 1) * T], kda_all[((h * 2 + m) * 4 + rr) * 32:((h * 2 + m) * 4 + rr + 1) * 32, 0:T], reads=[rD["kda_all"]], writes=[rk])
                    dma("sp", kt[32 * pg:32 * pg + 32, S:S + CT], kda_own[r0:r0 + 32, T:TT], reads=[rD["kda_own"]], writes=[rk])
                    dma("sp", qt[32 * pg:32 * pg + 32, 0, :], qda[r0:r0 + 32, :], reads=[rD["qda"]], writes=[rq])
                    dma("sp", qt[32 * pg:32 * pg + 32, 1, :], qdaf[r0:r0 + 32, :], reads=[rD["qdaf"]], writes=[rq])
                va, vo, nva, nvo = vda_all, vda_own, "vda_all", "vda_own"
                dk, scale = 32, 32 ** -0.5
            else:
                for rr in range(4):
                    for half in range(2):
                        q0 = ((2 * h + half) * 4 + rr) * 48
                        dma("sp", kt[half * 48:(half + 1) * 48, rr * T:(rr + 1) * T], kml_all[q0:q0 + 48, 0:T], reads=[rD["kml_all"]], writes=[rk])
                dma("sp", kt[0:96, S:S + CT], kml_own[h * 96:(h + 1) * 96, T:TT], reads=[rD["kml_own"]], writes=[rk])
                dma("sp", qt[0:96, 0, :], qml[h], reads=[rD["qml"]], writes=[rq])
                dma("sp", qt[0:96, 1, :], qmlf[h], reads=[rD["qmlf"]], writes=[rq])
                va, vo, nva, nvo = vml_all, vml_own, "vml_all", "vml_own"
                dk, scale = 96, 96 ** -0.5
            for rr in range(4):
                row0 = (h * 4 + rr) * TT
                dma("sp", vv[rr * 32:(rr + 1) * 32, 0:128, :], va[row0:row0 + T, :].rearrange("(p j) e -> p j e", j=128), reads=[rD[nva]], writes=[rv])
            dma("sp", vv[:, 128:130, :], vo[h * TT + T:(h + 1) * TT, :].rearrange("(j p) e -> p j e", p=128), reads=[rD[nvo]], writes=[rv])
            for gi, (t0, N, isctx) in enumerate(qgroups):
                ps_o, r_o = psb[6 + gi % 2], psr[6 + gi % 2]
                attend(kt, rk, vv, rv, qt, rq, dk, scale, t0, N, isctx, ps_o, r_o)
                o1, ro1 = normalise(ps_o, r_o, N)
                if kind == "da" and m == 0:
                    P.op("pool", lambda e: e.tensor_copy(out=o1all[:, t0:t0 + N], in_=o1[:, 0:N]), reads=[ro1], writes=[r_o1all])
                    continue
                yi = ctr["y"] % 2
                ctr["y"] += 1
                if kind == "da":
                    P.op("dve", lambda e: e.scalar_tensor_tensor(out=od[:, 0:N], in0=o1[:, 0:N], scalar=lsc[0:64, 3:4], in1=o1all[:, t0:t0 + N],
                                                                 op0=ALU.mult, op1=ALU.add), reads=[ro1, r_o1all, r_l], writes=[r_od])
                    P.op("pool", lambda e: e.tensor_tensor(out=od2[:, 0:N], in0=od[:, 0:N], in1=od[:, 0:N], op=ALU.mult), reads=[r_od], writes=[r_od])
                    ps, pr = ps_o, r_o
                    P.op("pe", lambda e: e.matmul(ps[0:64, 0:N], lhsT=ones[0:64, 0:64], rhs=od2[:, 0:N], start=True, stop=True),
                         reads=[r_od, r_const], writes=[pr])
                    P.op("act", lambda e: e.activation(out=od2[:, 0:N], in_=ps[0:64, 0:N], func=AF.Sqrt, scale=1.0 / 64, bias=epsr[0:64, 0:1]),
                         reads=[pr, r_const], writes=[r_od])
                    P.op("dve", lambda e: e.reciprocal(out=od2[:, 0:N], in_=od2[:, 0:N]), reads=[r_od], writes=[r_od])
                    P.op("dve", lambda e: e.scalar_tensor_tensor(out=yb[yi][:, 0:N], in0=od[:, 0:N], scalar=gsub[:, 0:1], in1=od2[:, 0:N],
                                                                 op0=ALU.mult, op1=ALU.mult), reads=[r_od, r_l], writes=[r_yb[yi]])
                    slot = h
                else:
                    P.op("dve", lambda e: e.tensor_copy(out=yb[yi][:, 0:N], in_=o1[:, 0:N]), reads=[ro1], writes=[r_yb[yi]])
                    slot = 6 + h
                dma("sp", yT[slot, :, t0:t0 + N], yb[yi][:, 0:N], reads=[r_yb[yi]], writes=[rY])
        P.barrier()
        P.flush()
        es.close()

    def stage_C(l, last):
        es = ExitStack()
        modT, r_mod = compute_mod(es, l)
        xn, cxn = xnames(l)
        xon, cxon = xnames(l + 1)
        yT, xT, cxT = dget("yT"), dget(xn), dget(cxn)
        xTn = dget(xon)
        cxTn = None if last else dget(cxon)
        x1T = cx.dram("x1T_%d" % l, [8, 128, TT], F32)
        h2T = cx.dram("h2T_%d" % l, [8, 128, TT], BF16)
        gTd = cx.dram("gT_%d" % l, [16, TT], F32)
        rD = cx.res
        lnv = sbuf(es, "lnv", [128, 4, 8], F32)
        rw = sbuf(es, "rw", [128, 8, 16], F32)
        rb = sbuf(es, "rb", [128, 16], F32)
        selE = sbuf(es, "selE", [16, 16, 128], F32)
        r_c = Res()
        for i, nm in enumerate(("ln1_gT", "ln1_bT", "ln2_gT", "ln2_bT")):
            dma("sp", lnv[:, i, :], W[nm][l], writes=[r_c])
        dma("sp", rw[:], W["router_w"].rearrange("(c p) n -> p c n", p=128), writes=[r_c])
        dma("sp", rb[:], W["router_b"].partition_broadcast(128), writes=[r_c])
        for e_ in range(16):
            P.op("dve", lambda e, e_=e_: e.tensor_scalar_mul(out=selE[:, e_, :], in0=ones[0:16, :], scalar1=ident[0:16, e_:e_ + 1]),
                 reads=[r_const], writes=[r_c])
        groups = GROUPS[:8] + ([] if last else [GROUPS[8]])

        def layer_norm(u, r_u, N, gi_, bi_, outt, r_out, tmpA, tmpB, r_t):
            sqv = tmpA
            for c in range(8):
                P.op("pool", lambda e, c=c: e.tensor_tensor(out=sqv[:, c, 0:N], in0=u[:, c, 0:N], in1=u[:, c, 0:N], op=ALU.mult),
                     reads=[r_u], writes=[r_t])
            p1, r1 = next_ps()
            for c in range(8):
                P.op("pe", lambda e, c=c: e.matmul(p1[:, 0:N], lhsT=ones[:, :], rhs=u[:, c, 0:N], start=(c == 0), stop=(c == 7)),
                     reads=[r_u, r_const], writes=[r1])
            p2, r2 = next_ps()
            for c in range(8):
                P.op("pe", lambda e, c=c: e.matmul(p2[:, 0:N], lhsT=ones[:, :], rhs=sqv[:, c, 0:N], start=(c == 0), stop=(c == 7)),
                     reads=[r_t, r_const], writes=[r2])
            mean, msq, rs = tmpB[:, 0, :], tmpB[:, 1, :], tmpB[:, 2, :]
            r_b = Res()
            P.op("dve", lambda e: e.tensor_scalar_mul(out=mean[:, 0:N], in0=p1[:, 0:N], scalar1=1.0 / D), reads=[r1], writes=[r_b])
            P.op("pool", lambda e: e.tensor_tensor(out=msq[:, 0:N], in0=mean[:, 0:N], in1=mean[:, 0:N], op=ALU.mult), reads=[r_b], writes=[r_b])
            P.op("dve", lambda e: e.scalar_tensor_tensor(out=rs[:, 0:N], in0=p2[:, 0:N], scalar=1.0 / D, in1=msq[:, 0:N],
                                                         op0=ALU.mult, op1=ALU.subtract), reads=[r2, r_b], writes=[r_b])
            P.op("act", lambda e: e.activation(out=rs[:, 0:N], in_=rs[:, 0:N], func=AF.Sqrt, bias=epsr[:, 1:2], scale=1.0),
                 reads=[r_b, r_const], writes=[r_b])
            P.op("dve", lambda e: e.reciprocal(out=rs[:, 0:N], in_=rs[:, 0:N]), reads=[r_b], writes=[r_b])
            for c in range(8):
                P.op("pool", lambda e, c=c: e.tensor_tensor(out=sqv[:, c, 0:N], in0=u[:, c, 0:N], in1=mean[:, 0:N], op=ALU.subtract),
                     reads=[r_u, r_b, r_t], writes=[r_t])
                P.op("dve", lambda e, c=c: e.tensor_tensor(out=sqv[:, c, 0:N], in0=sqv[:, c, 0:N], in1=rs[:, 0:N], op=ALU.mult),
                     reads=[r_t, r_b], writes=[r_t])
                P.op("dve", lambda e, c=c: e.tensor_scalar(out=outt[:, c, 0:N], in0=sqv[:, c, 0:N], scalar1=lnv[:, gi_, c:c + 1],
                                                          scalar2=lnv[:, bi_, c:c + 1], op0=ALU.mult, op1=ALU.add),
                     reads=[r_t, r_c], writes=[r_out])

        with ExitStack() as e1:
            wout = sbuf(e1, "wout", [64, 16, D], BF16)
            stgo = [sbuf(e1, "stgo%d" % i, [64, D], F32) for i in range(2)]
            r_wo, r_so = Res(), [Res(), Res()]
            wov = W["w_out"][l].rearrange("(kt p) n -> p kt n", p=64)
            for kt in range(16):
                dma("sp", stgo[kt % 2][:], wov[:, kt, :], writes=[r_so[kt % 2]])
                P.op("dve", lambda e, kt=kt: e.tensor_copy(out=wout[:, kt, :], in_=stgo[kt % 2][:]), reads=[r_so[kt % 2]], writes=[r_wo])
            yg = [sbuf(e1, "yg%d" % i, [64, 16, 512], BF16) for i in range(2)]
            xg = [sbuf(e1, "xgc%d" % i, [128, 8, 512], F32) for i in range(2)]
            r_yg, r_xg = [Res(), Res()], [Res(), Res()]
            u = sbuf(e1, "u", [128, 8, 512], F32)
            tA = sbuf(e1, "tA", [128, 8, 512], F32)
            tB = sbuf(e1, "tB", [128, 3, 512], F32)
            x1 = sbuf(e1, "x1", [128, 8, 512], F32)
            h2f = sbuf(e1, "h2f", [128, 8, 512], F32)
            h2b = sbuf(e1, "h2b", [128, 8, 512], BF16)
            gts = sbuf(e1, "gts", [16, 512], F32)
            rt = sbuf(e1, "rt", [128, 160], F32)
            r_u, r_tA, r_x1, r_h2f, r_h2b, r_gts, r_rt = Res(), Res(), Res(), Res(), Res(), Res(), Res()
            for gi, (t0, N) in enumerate(groups):
                b = gi % 2
                isctx = (t0 == T)
                mi = 1 if isctx else 0
                dma("sp", yg[b][:, :, 0:N], yT.rearrange("s p t -> p s t")[:, :, t0:t0 + N], reads=[rD["yT"]], writes=[r_yg[b]])
                src = cxT.rearrange("c p t -> p c t") if isctx else xT.rearrange("c p t -> p c t")[:, :, t0:t0 + N]
                dma("sp", xg[b][:, :, 0:N], src, reads=[rD[cxn if isctx else xn]], writes=[r_xg[b]])
                P.op("pool", lambda e: e.tensor_scalar_mul(out=xg[b][:, :, 0:N], in0=xg[b][:, :, 0:N], scalar1=float(ALPHA)),
                     reads=[r_xg[b]], writes=[r_xg[b]])
                for oc in range(8):
                    ps, pr = next_ps()
                    for kt in range(16):
                        P.op("pe", lambda e, kt=kt: e.matmul(ps[:, 0:N], lhsT=wout[:, kt, oc * 128:(oc + 1) * 128], rhs=yg[b][:, kt, 0:N],
                                                            start=(kt == 0), stop=(kt == 15)), reads=[r_wo, r_yg[b]], writes=[pr])
                    P.op("dve", lambda e: e.scalar_tensor_tensor(out=u[:, oc, 0:N], in0=ps[:, 0:N], scalar=modT[:, 16 + oc, mi:mi + 1],
                                                                 in1=xg[b][:, oc, 0:N], op0=ALU.mult, op1=ALU.add),
                         reads=[pr, r_mod, r_xg[b]], writes=[r_u])
                layer_norm(u, r_u, N, 0, 1, x1, r_x1, tA, tB, r_tA)
                dma("act", x1T.rearrange("c p t -> p c t")[:, :, t0:t0 + N], x1[:, :, 0:N], reads=[r_x1], writes=[rD["x1T_%d" % l]])
                for c in range(8):
                    P.op("dve", lambda e, c=c: e.tensor_scalar(out=h2f[:, c, 0:N], in0=x1[:, c, 0:N], scalar1=modT[:, 32 + c, mi:mi + 1],
                                                              scalar2=modT[:, 24 + c, mi:mi + 1], op0=ALU.mult, op1=ALU.add),
                         reads=[r_x1, r_mod], writes=[r_h2f])
                    P.op("pool", lambda e, c=c: e.tensor_copy(out=h2b[:, c, 0:N], in_=h2f[:, c, 0:N]), reads=[r_h2f], writes=[r_h2b])
                dma("act", h2T.rearrange("c p t -> p c t")[:, :, t0:t0 + N], h2b[:, :, 0:N], reads=[r_h2b], writes=[rD["h2T_%d" % l]])
                for j in range(N // 128):
                    ts = slice(j * 128, (j + 1) * 128)
                    ps, pr = next_ps()
                    for c in range(8):
                        P.op("pe", lambda e, c=c: e.matmul(ps[:, 0:16], lhsT=h2f[:, c, ts], rhs=rw[:, c, :], start=(c == 0), stop=(c == 7)),
                             reads=[r_h2f, r_c], writes=[pr])
                    sc_, ch, mc, sel1, tmpv = rt[:, 0:16], rt[:, 16:32], rt[:, 32:48], rt[:, 48:64], rt[:, 64:80]
                    q4 = rt[:, 80:112].rearrange("p (a b) -> p a b", a=8)
                    s1 = rt[:, 112:120]
                    ch4 = ch.rearrange("p (g i) -> p g i", g=4)
                    mc4 = mc.rearrange("p (g i) -> p g i", g=4)
                    R_ = [r_rt]

                    def dv(fn):
                        P.op("dve", fn, reads=R_ + [r_c], writes=R_)
                    P.op("act", lambda e: e.activation(out=sc_, in_=ps[:, 0:16], func=AF.Sigmoid), reads=[pr], writes=R_)
                    dv(lambda e: e.tensor_tensor(out=ch, in0=sc_, in1=rb[:, :], op=ALU.add))
                    dv(lambda e: e.tensor_tensor(out=q4[:, 0, :], in0=ch4[:, :, 0], in1=ch4[:, :, 1], op=ALU.max))
                    dv(lambda e: e.tensor_tensor(out=q4[:, 1, :], in0=ch4[:, :, 0], in1=ch4[:, :, 1], op=ALU.min))
                    dv(lambda e: e.tensor_tensor(out=q4[:, 2, :], in0=ch4[:, :, 2], in1=ch4[:, :, 3], op=ALU.max))
                    dv(lambda e: e.tensor_tensor(out=q4[:, 3, :], in0=ch4[:, :, 2], in1=ch4[:, :, 3], op=ALU.min))
                    dv(lambda e: e.tensor_tensor(out=q4[:, 4, :], in0=q4[:, 0, :], in1=q4[:, 2, :], op=ALU.max))
                    dv(lambda e: e.tensor_tensor(out=q4[:, 5, :], in0=q4[:, 0, :], in1=q4[:, 2, :], op=ALU.min))
                    dv(lambda e: e.tensor_tensor(out=q4[:, 6, :], in0=q4[:, 1, :], in1=q4[:, 3, :], op=ALU.max))
                    dv(lambda e: e.tensor_tensor(out=q4[:, 5, :], in0=q4[:, 5, :], in1=q4[:, 6, :], op=ALU.max))
                    dv(lambda e: e.tensor_tensor(out=q4[:, 4, :], in0=q4[:, 4, :], in1=q4[:, 5, :], op=ALU.add))
                    dv(lambda e: e.reduce_max(out=s1[:, 0:1], in_=q4[:, 4, :], axis=AX.X))
                    dv(lambda e: e.tensor_scalar(out=q4[:, 7, :], in0=q4[:, 4, :], scalar1=s1[:, 0:1], scalar2=None, op0=ALU.is_equal))
                    dv(lambda e: e.tensor_scalar(out=q4[:, 7, :], in0=q4[:, 7, :], scalar1=-1.0, scalar2=1.0e9, op0=ALU.add, op1=ALU.mult))
                    for i in range(4):
                        dv(lambda e, i=i: e.tensor_tensor(out=mc4[:, :, i], in0=ch4[:, :, i], in1=q4[:, 7, :], op=ALU.add))
                    dv(lambda e: e.reduce_max(out=s1[:, 1:2], in_=mc, axis=AX.X))
                    dv(lambda e: e.tensor_scalar(out=sel1, in0=mc, scalar1=s1[:, 1:2], scalar2=None, op0=ALU.is_equal))
                    dv(lambda e: e.scalar_tensor_tensor(out=mc, in0=sel1, scalar=-1.0e9, in1=mc, op0=ALU.mult, op1=ALU.add))
                    dv(lambda e: e.reduce_max(out=s1[:, 2:3], in_=mc, axis=AX.X))
                    dv(lambda e: e.tensor_scalar(out=tmpv, in0=mc, scalar1=s1[:, 2:3], scalar2=None, op0=ALU.is_equal))
                    dv(lambda e: e.tensor_tensor(out=sel1, in0=sel1, in1=tmpv, op=ALU.add))
                    dv(lambda e: e.tensor_tensor(out=sel1, in0=sel1, in1=sc_, op=ALU.mult))
                    dv(lambda e: e.reduce_sum(out=s1[:, 3:4], in_=sel1, axis=AX.X))
                    dv(lambda e: e.reciprocal(out=s1[:, 3:4], in_=s1[:, 3:4]))
                    dv(lambda e: e.tensor_scalar_mul(out=sel1, in0=sel1, scalar1=s1[:, 3:4]))
                    ps2, pr2 = next_ps()
                    P.op("pe", lambda e: e.transpose(ps2[0:16, 0:128], sel1, ident[:, :]), reads=R_ + [r_const], writes=[pr2])
                    P.op("dve", lambda e: e.tensor_copy(out=gts[:, ts], in_=ps2[0:16, 0:128]), reads=[pr2], writes=[r_gts])
                dma("act", gTd[:, t0:t0 + N], gts[:, 0:N], reads=[r_gts], writes=[rD["gT_%d" % l]])
            P.barrier()

        blocks = [(0, 1024), (1024, 1024), (2048, 1024), (3072, 1024 if last else 1024 + CT)]
        NBM = 1024 + CT
        with ExitStack() as e2:
            h2k = sbuf(e2, "h2k", [128, 8, NBM], BF16)
            gk = sbuf(e2, "gk", [16, NBM], F32)
            acc = sbuf(e2, "acc", [128, 8, NBM], F32)
            r_h2k, r_gk, r_acc = Res(), Res(), Res()
            for (b0, NB) in blocks:
                subs = [(o, min(512, NB - o)) for o in range(0, NB, 512)]
                dma("sp", h2k[:, :, 0:NB], h2T.rearrange("c p t -> p c t")[:, :, b0:b0 + NB], reads=[rD["h2T_%d" % l]], writes=[r_h2k])
                dma("sp", gk[:, 0:NB], gTd[:, b0:b0 + NB], reads=[rD["gT_%d" % l]], writes=[r_gk])
                with ExitStack() as eE:
                    w1b = [sbuf(eE, "w1b%d" % i, [128, 8, 512], BF16) for i in range(2)]
                    w3b = [sbuf(eE, "w3b%d" % i, [128, 8, 512], BF16) for i in range(2)]
                    w2b = [sbuf(eE, "w2b%d" % i, [128, 4, D], BF16) for i in range(2)]
                    r_w1, r_w3, r_w2 = [Res(), Res()], [Res(), Res()], [Res(), Res()]
                    gbc = [sbuf(eE, "gbc%d" % i, [128, 512], F32) for i in range(2)]
                    sa = [sbuf(eE, "sa%d" % i, [128, 512], F32) for i in range(4)]
                    gb = [sbuf(eE, "gb%d" % i, [128, 512], F32) for i in range(4)]
                    gT_ = [sbuf(eE, "gT_%d" % i, [128, 4, 512], BF16) for i in range(2)]
                    r_gbc, r_gT = [Res(), Res()], [Res(), Res()]
                    r_sa, r_gb = [Res() for _ in range(4)], [Res() for _ in range(4)]
                    cnt4 = {"i": 0}

                    def wload(e_):
                        k = e_ % 2
                        dma("pool", w1b[k][:], W["exp_w1"][l, e_].rearrange("(c p) n -> p c n", p=128), writes=[r_w1[k]])
                        dma("pool", w3b[k][:], W["exp_w3"][l, e_].rearrange("(c p) n -> p c n", p=128), writes=[r_w3[k]])
                        dma("pool", w2b[k][:], W["exp_w2"][l, e_].rearrange("(c p) n -> p c n", p=128), writes=[r_w2[k]])

                    items = [(e_, o, n) for e_ in range(NE) for (o, n) in subs]

                    def s1(i):
                        e_, o, n = items[i]
                        k = i % 2
                        kw_ = e_ % 2
                        ps, pr = next_ps(0, 8)
                        P.op("pe", lambda e: e.matmul(ps[:, 0:n], lhsT=selE[:, e_, :], rhs=gk[:, o:o + n], start=True, stop=True),
                             reads=[r_gk, r_c], writes=[pr])
                        P.op("act", lambda e: e.copy(out=gbc[k][:, 0:n], in_=ps[:, 0:n]), reads=[pr], writes=[r_gbc[k]])
                        for fc in range(4):
                            q_ = cnt4["i"] % 4
                            cnt4["i"] += 1
                            pa, ra = next_ps(0, 8)
                            for c in range(8):
                                P.op("pe", lambda e, c=c: e.matmul(pa[:, 0:n], lhsT=w1b[kw_][:, c, fc * 128:(fc + 1) * 128], rhs=h2k[:, c, o:o + n],
                                                                  start=(c == 0), stop=(c == 7)), reads=[r_w1[kw_], r_h2k], writes=[ra])
                            pb, rb_ = next_ps(0, 8)
                            for c in range(8):
                                P.op("pe", lambda e, c=c: e.matmul(pb[:, 0:n], lhsT=w3b[kw_][:, c, fc * 128:(fc + 1) * 128], rhs=h2k[:, c, o:o + n],
                                                                  start=(c == 0), stop=(c == 7)), reads=[r_w3[kw_], r_h2k], writes=[rb_])
                            P.op("act", lambda e: e.activation(out=sa[q_][:, 0:n], in_=pa[:, 0:n], func=AF.Silu), reads=[ra], writes=[r_sa[q_]])
                            P.op("dve", lambda e: e.tensor_tensor(out=gb[q_][:, 0:n], in0=pb[:, 0:n], in1=gbc[k][:, 0:n], op=ALU.mult),
                                 reads=[rb_, r_gbc[k]], writes=[r_gb[q_]])
                            P.op("pool", lambda e: e.tensor_tensor(out=gT_[k][:, fc, 0:n], in0=sa[q_][:, 0:n], in1=gb[q_][:, 0:n], op=ALU.mult),
                                 reads=[r_sa[q_], r_gb[q_]], writes=[r_gT[k]])

                    def s2(i):
                        e_, o, n = items[i]
                        k = i % 2
                        kw_ = e_ % 2
                        for oc in range(8):
                            pf, rf = next_ps(0, 8)
                            for fc in range(4):
                                P.op("pe", lambda e, fc=fc: e.matmul(pf[:, 0:n], lhsT=w2b[kw_][:, fc, oc * 128:(oc + 1) * 128], rhs=gT_[k][:, fc, 0:n],
                                                                    start=(fc == 0), stop=(fc == 3)), reads=[r_w2[kw_], r_gT[k]], writes=[rf])
                            if e_ == 0:
                                P.op("dve", lambda e: e.tensor_copy(out=acc[:, oc, o:o + n], in_=pf[:, 0:n]), reads=[rf], writes=[r_acc])
                            else:
                                P.op("dve", lambda e: e.tensor_tensor(out=acc[:, oc, o:o + n], in0=acc[:, oc, o:o + n], in1=pf[:, 0:n], op=ALU.add),
                                     reads=[rf, r_acc], writes=[r_acc])
                    wload(0)
                    wload(1)
                    s1(0)
                    for i in range(len(items)):
                        if i + 1 < len(items):
                            s1(i + 1)
                        s2(i)
                        e_cur = items[i][0]
                        if (i + 1 == len(items) or items[i + 1][0] != e_cur) and e_cur + 2 < NE:
                            wload(e_cur + 2)
                    P.barrier()
                with ExitStack() as e3:
                    x1g = sbuf(e3, "x1g", [128, 8, 512], F32)
                    tA2 = sbuf(e3, "tA2", [128, 8, 512], F32)
                    tB2 = sbuf(e3, "tB2", [128, 3, 512], F32)
                    xo = sbuf(e3, "xo", [128, 8, 512], F32)
                    r_x1g, r_tA2, r_xo = Res(), Res(), Res()
                    u2, r_u2 = x1g, r_x1g
                    for (o, n) in subs:
                        t0 = b0 + o
                        isctx = (t0 >= T)
                        mi = 1 if isctx else 0
                        dma("sp", x1g[:, :, 0:n], x1T.rearrange("c p t -> p c t")[:, :, t0:t0 + n], reads=[rD["x1T_%d" % l]], writes=[r_x1g])
                        P.op("pool", lambda e: e.tensor_scalar_mul(out=x1g[:, :, 0:n], in0=x1g[:, :, 0:n], scalar1=float(ALPHA)),
                             reads=[r_x1g], writes=[r_x1g])
                        for c in range(8):
                            P.op("dve", lambda e, c=c: e.scalar_tensor_tensor(out=u2[:, c, 0:n], in0=acc[:, c, o:o + n], scalar=modT[:, 40 + c, mi:mi + 1],
                                                                             in1=x1g[:, c, 0:n], op0=ALU.mult, op1=ALU.add),
                                 reads=[r_acc, r_mod, r_x1g], writes=[r_u2])
                        layer_norm(u2, r_u2, n, 2, 3, xo, r_xo, tA2, tB2, r_tA2)
                        if isctx:
                            dma("act", cxTn.rearrange("c p t -> p c t"), xo[:, :, 0:n], reads=[r_xo], writes=[rD[cxon]])
                        else:
                            dma("act", xTn.rearrange("c p t -> p c t")[:, :, t0:t0 + n], xo[:, :, 0:n], reads=[r_xo], writes=[rD[xon]])
                    P.barrier()
        P.flush()
        es.close()

    def stage_R(l, last):
        es = ExitStack()
        lg, r_lg = load_lg(es, l)
        yT = dget("yT")
        rD = cx.res
        qr, kr, krt, vr, sg, rsum_all = dget("qr"), dget("kr"), dget("krt"), dget("vr"), dget("sg"), dget("rsum_all")
        NCH = TT // 128
        cst = sbuf(es, "cst", [128, 4, 128], F32)
        tpos = sbuf(es, "tposr", [128, 32], F32)
        eb = sbuf(es, "eb", [128, 16], F32)
        r_k = Res()
        dma("sp", cst[:], W["rtabs"].rearrange("a p c -> p a c"), writes=[r_k])
        dma("sp", tpos[:], W["tpos"][:, :], writes=[r_k])
        dma("sp", eb[:], W["ebound"][:, :], writes=[r_k])
        MT = sbuf(es, "MT", [128, 128], F32)
        xi = sbuf(es, "xi", [64, 2, 128], F32)
        zeta = sbuf(es, "zeta", [128, 4], F32)
        zc = sbuf(es, "zc", [128, 4], F32)
        cdec = sbuf(es, "cdec", [64, 2], F32)
        coef = sbuf(es, "coef", [64, 16], F32)
        tmpm = sbuf(es, "tmpm", [128, 128], F32)
        p127 = sbuf(es, "p127", [128, 4], F32)
        qT = sbuf(es, "qTr", [64, TT], BF16)
        kT = sbuf(es, "kTr", [64, TT], BF16)
        ktm = sbuf(es, "ktm", [128, NCH, 64], BF16)
        vtm = sbuf(es, "vtm", [128, NCH, 64], BF16)
        kz = sbuf(es, "kz", [128, NCH, 2, 64], BF16)
        sgT = sbuf(es, "sgT", [64, TT], BF16)
        rs_all = sbuf(es, "rs_all", [64, 4, 512], F32)
        Pst = sbuf(es, "Pst", [64, NCH + 1, 64], F32)
        Nst = sbuf(es, "Nst", [64, NCH + 1, 64], F32)
        Pb = sbuf(es, "Pb", [64, NCH + 1, 64], BF16)
        Nb = sbuf(es, "Nb", [64, NCH + 1, 64], BF16)
        am = [sbuf(es, "am%d" % i, [128, 128], BF16) for i in range(2)]
        qx = [sbuf(es, "qx%d" % i, [64, 2, 128], BF16) for i in range(2)]
        osb_ = sbuf(es, "osbr", [64, 512], F32)
        osq = sbuf(es, "osq", [64, 512], F32)
        yb = sbuf(es, "ybr", [64, 512], BF16)
        r_q, r_kk, r_ktm, r_vtm, r_kz, r_sg, r_rs, r_st, r_o, r_yb = (Res() for _ in range(10))
        r_am, r_qx = [Res(), Res()], [Res(), Res()]
        dma("sp", rs_all[:], rsum_all.rearrange("(r p) c -> p r c", p=64), reads=[rD["rsum_all"]], writes=[r_rs])
        P.op("dve", lambda e: e.tensor_scalar(out=p127[:, 0:1], in0=tpos[:, 0:1], scalar1=-1.0, scalar2=127.0, op0=ALU.mult, op1=ALU.add), reads=[r_k], writes=[r_k])
        P.op("dve", lambda e: e.tensor_scalar(out=p127[:, 2:3], in0=tpos[:, 0:1], scalar1=-1.0, scalar2=255.0, op0=ALU.mult, op1=ALU.add), reads=[r_k], writes=[r_k])
        for h in range(4):
            lf, lb = lg[:, h:h + 1], lg[:, 4 + h:5 + h]
            RK = [r_k, r_lg]
            P.op("dve", lambda e: e.tensor_scalar_mul(out=tmpm[:], in0=cst[:, 0, :], scalar1=lf), reads=RK, writes=[r_k])
            P.op("dve", lambda e: e.scalar_tensor_tensor(out=tmpm[:], in0=cst[:, 1, :], scalar=lb, in1=tmpm[:], op0=ALU.mult, op1=ALU.add), reads=RK, writes=[r_k])
            P.op("act", lambda e: e.activation(out=MT[:], in_=tmpm[:], func=AF.Exp), reads=RK, writes=[r_k])
            P.op("act", lambda e: e.activation(out=xi[:, 0, :], in_=cst[0:64, 2, :], func=AF.Exp, scale=lg[0:64, h:h + 1]), reads=RK, writes=[r_k])
            P.op("act", lambda e: e.activation(out=xi[:, 1, :], in_=cst[0:64, 3, :], func=AF.Exp, scale=lg[0:64, 4 + h:5 + h]), reads=RK, writes=[r_k])
            P.op("act", lambda e: e.activation(out=zeta[:, 0:1], in_=p127[:, 0:1], func=AF.Exp, scale=lf), reads=RK, writes=[r_k])
            P.op("act", lambda e: e.activation(out=zeta[:, 1:2], in_=tpos[:, 0:1], func=AF.Exp, scale=lb), reads=RK, writes=[r_k])
            P.op("act", lambda e: e.activation(out=zc[:, 0:1], in_=p127[:, 2:3], func=AF.Exp, scale=lf), reads=RK, writes=[r_k])
            P.op("act", lambda e: e.activation(out=zc[:, 1:2], in_=p127[:, 0:1], func=AF.Exp, scale=lf), reads=RK, writes=[r_k])
            P.op("act", lambda e: e.activation(out=zc[:, 2:4], in_=tpos[:, 0:2], func=AF.Exp, scale=lb), reads=RK, writes=[r_k])
            P.op("act", lambda e: e.activation(out=coef[:, 0:16], in_=eb[0:64, :], func=AF.Exp, scale=lg[0:64, h:h + 1]), reads=RK, writes=[r_k])
            P.op("act", lambda e: e.activation(out=coef[:, 4:8], in_=eb[0:64, 4:8], func=AF.Exp, scale=lg[0:64, 4 + h:5 + h]), reads=RK, writes=[r_k])
            P.op("act", lambda e: e.activation(out=coef[:, 9:10], in_=eb[0:64, 9:10], func=AF.Exp, scale=lg[0:64, 4 + h:5 + h]), reads=RK, writes=[r_k])
            P.op("act", lambda e: e.activation(out=cdec[:, 0:1], in_=cst[0:64, 3, 0:1], func=AF.Exp, scale=lg[0:64, h:h + 1]), reads=RK, writes=[r_k])
            P.op("act", lambda e: e.activation(out=cdec[:, 1:2], in_=cst[0:64, 3, 0:1], func=AF.Exp, scale=lg[0:64, 4 + h:5 + h]), reads=RK, writes=[r_k])
            dma("sp", qT[:], qr[h], reads=[rD["qr"]], writes=[r_q])
            dma("sp", kT[:], kr[h], reads=[rD["kr"]], writes=[r_kk])
            dma("sp", sgT[:], sg[h], reads=[rD["sg"]], writes=[r_sg])
            dma("sp", ktm[:], krt.rearrange("(j p) c -> p j c", p=128)[:, :, h * 64:(h + 1) * 64], reads=[rD["krt"]], writes=[r_ktm])
            dma("sp", vtm[:], vr.rearrange("(j p) c -> p j c", p=128)[:, :, h * 64:(h + 1) * 64], reads=[rD["vr"]], writes=[r_vtm])
            for d_ in range(2):
                P.op("pool", lambda e, d_=d_: e.tensor_scalar_mul(out=kz[:, :, d_, :], in0=ktm[:], scalar1=zeta[:, d_:d_ + 1]),
                     reads=[r_ktm, r_k], writes=[r_kz])
            kzc = sbuf(es, "kzc%d" % h, [128, 2, 2, 64], BF16)
            r_kzc = Res()
            for jt in range(2):
                P.op("pool", lambda e, jt=jt: e.tensor_scalar_mul(out=kzc[:, jt, 0, :], in0=ktm[:, 32 + jt, :], scalar1=zc[:, jt:jt + 1]), reads=[r_ktm, r_k], writes=[r_kzc])
                P.op("pool", lambda e, jt=jt: e.tensor_scalar_mul(out=kzc[:, jt, 1, :], in0=ktm[:, 32 + jt, :], scalar1=zc[:, 2 + jt:3 + jt]), reads=[r_ktm, r_k], writes=[r_kzc])
            pc, rc = next_ps()
            for d_ in range(2):
                for jt in range(2):
                    P.op("pe", lambda e, d_=d_, jt=jt: e.matmul(pc[0:64, d_ * 64:(d_ + 1) * 64], lhsT=kzc[:, jt, d_, :], rhs=vtm[:, 32 + jt, :],
                                                               start=(jt == 0), stop=(jt == 1)), reads=[r_kzc, r_vtm], writes=[rc])
            RS = [r_rs, r_k, r_st]
            P.op("dve", lambda e: e.tensor_scalar_mul(out=Pst[:, 0, :], in0=pc[0:64, 0:64], scalar1=coef[:, 8:9]), reads=[rc] + RS, writes=[r_st])
            P.op("dve", lambda e: e.tensor_scalar_mul(out=Nst[:, 32, :], in0=pc[0:64, 64:128], scalar1=coef[:, 9:10]), reads=[rc] + RS, writes=[r_st])
            for rp in range(4):
                P.op("dve", lambda e, rp=rp: e.scalar_tensor_tensor(out=Pst[:, 0, :], in0=rs_all[:, rp, h * 64:(h + 1) * 64], scalar=coef[:, rp:rp + 1],
                                                                   in1=Pst[:, 0, :], op0=ALU.mult, op1=ALU.add), reads=RS, writes=[r_st])
                P.op("dve", lambda e, rp=rp: e.scalar_tensor_tensor(out=Nst[:, 32, :], in0=rs_all[:, rp, (4 + h) * 64:(5 + h) * 64], scalar=coef[:, 4 + rp:5 + rp],
                                                                   in1=Nst[:, 32, :], op0=ALU.mult, op1=ALU.add), reads=RS, writes=[r_st])
            for n0 in range(0, 32, 4):
                pu, ru = next_ps()
                for n in range(n0, n0 + 4):
                    for d_ in range(2):
                        cc = ((n - n0) * 2 + d_) * 64
                        P.op("pe", lambda e, n=n, d_=d_, cc=cc: e.matmul(pu[0:64, cc:cc + 64], lhsT=kz[:, n, d_, :], rhs=vtm[:, n, :], start=True, stop=True),
                             reads=[r_kz, r_vtm], writes=[ru])
                P.op("dve", lambda e, n0=n0: e.tensor_copy(out=osq[:, 0:512], in_=pu[0:64, 0:512]), reads=[ru, r_o], writes=[r_o])
                for n in range(n0, n0 + 4):
                    cc = (n - n0) * 128
                    P.op("pool", lambda e, n=n, cc=cc: e.tensor_copy(out=Pst[:, n + 1, :], in_=osq[:, cc:cc + 64]), reads=[r_o, r_st], writes=[r_st])
                    P.op("pool", lambda e, n=n, cc=cc: e.tensor_copy(out=Nst[:, n, :], in_=osq[:, cc + 64:cc + 128]), reads=[r_o, r_st], writes=[r_st])
            for n in range(32):
                if n < 31:
                    P.op("dve", lambda e, n=n: e.scalar_tensor_tensor(out=Pst[:, n + 1, :], in0=Pst[:, n, :], scalar=cdec[:, 0:1], in1=Pst[:, n + 1, :],
                                                                     op0=ALU.mult, op1=ALU.add), reads=[r_st, r_k], writes=[r_st])
            for n in range(31, 0, -1):
                P.op("dve", lambda e, n=n: e.scalar_tensor_tensor(out=Nst[:, n, :], in0=Nst[:, n + 1, :], scalar=cdec[:, 1:2], in1=Nst[:, n, :],
                                                                 op0=ALU.mult, op1=ALU.add), reads=[r_st, r_k], writes=[r_st])
            pu, ru = next_ps()
            P.op("pe", lambda e: e.matmul(pu[0:64, 0:64], lhsT=kz[:, 32, 0, :], rhs=vtm[:, 32, :], start=True, stop=True), reads=[r_kz, r_vtm], writes=[ru])
            P.op("pe", lambda e: e.matmul(pu[0:64, 64:128], lhsT=kz[:, 33, 1, :], rhs=vtm[:, 33, :], start=True, stop=True), reads=[r_kz, r_vtm], writes=[ru])
            P.op("pool", lambda e: e.memset(Pb[:, 32, :], 0.0), reads=[r_st], writes=[r_st])
            P.op("pool", lambda e: e.memset(Nb[:, 34, :], 0.0), reads=[r_st], writes=[r_st])
            P.op("dve", lambda e: e.tensor_copy(out=Pb[:, 33, :], in_=pu[0:64, 0:64]), reads=[ru, r_st], writes=[r_st])
            P.op("dve", lambda e: e.tensor_copy(out=Nb[:, 33, :], in_=pu[0:64, 64:128]), reads=[ru, r_st], writes=[r_st])
            P.op("dve", lambda e: e.tensor_copy(out=Pb[:, 0:32, :], in_=Pst[:, 0:32, :]), reads=[r_st], writes=[r_st])
            P.op("dve", lambda e: e.tensor_copy(out=Nb[:, 1:33, :], in_=Nst[:, 1:33, :]), reads=[r_st], writes=[r_st])
            nchunks = 32 if last else 34
            for n0 in range(0, nchunks, 4):
                nn = min(4, nchunks - n0)
                N = nn * 128
                po, ro = next_ps()
                for n in range(n0, n0 + nn):
                    k = n % 2
                    tsl = slice(n * 128, (n + 1) * 128)
                    pa, ra = next_ps()
                    P.op("pe", lambda e: e.matmul(pa[:, 0:128], lhsT=kT[:, tsl], rhs=qT[:, tsl], start=True, stop=True), reads=[r_kk, r_q], writes=[ra])
                    P.op("dve", lambda e: e.tensor_tensor(out=am[k][:], in0=pa[:, 0:128], in1=MT[:], op=ALU.mult), reads=[ra, r_k], writes=[r_am[k]])
                    P.op("pool", lambda e: e.tensor_tensor(out=qx[k][:, 0, :], in0=qT[:, tsl], in1=xi[:, 0, :], op=ALU.mult), reads=[r_q, r_k], writes=[r_qx[k]])
                    P.op("pool", lambda e: e.tensor_tensor(out=qx[k][:, 1, :], in0=qT[:, tsl], in1=xi[:, 1, :], op=ALU.mult), reads=[r_q, r_k], writes=[r_qx[k]])
                    oc = (n - n0) * 128
                    P.op("pe", lambda e: e.matmul(po[0:64, oc:oc + 128], lhsT=vtm[:, n, :], rhs=am[k][:], start=True, stop=False), reads=[r_vtm, r_am[k]], writes=[ro])
                    P.op("pe", lambda e: e.matmul(po[0:64, oc:oc + 128], lhsT=Pb[:, n, :], rhs=qx[k][:, 0, :], start=False, stop=False), reads=[r_st, r_qx[k]], writes=[ro])
                    P.op("pe", lambda e: e.matmul(po[0:64, oc:oc + 128], lhsT=Nb[:, n + 1, :], rhs=qx[k][:, 1, :], start=False, stop=True), reads=[r_st, r_qx[k]], writes=[ro])
                t0 = n0 * 128
                P.op("dve", lambda e: e.tensor_copy(out=osb_[:, 0:N], in_=po[0:64, 0:N]), reads=[ro, r_o], writes=[r_o])
                P.op("pool", lambda e: e.tensor_tensor(out=osq[:, 0:N], in0=osb_[:, 0:N], in1=osb_[:, 0:N], op=ALU.mult), reads=[r_o], writes=[r_o])
                pq, rq_ = next_ps()
                P.op("pe", lambda e: e.matmul(pq[0:64, 0:N], lhsT=ones[0:64, 0:64], rhs=osq[:, 0:N], start=True, stop=True), reads=[r_o, r_const], writes=[rq_])
                P.op("act", lambda e: e.activation(out=osq[:, 0:N], in_=pq[0:64, 0:N], func=AF.Sqrt, scale=1.0 / 64, bias=epsr[0:64, 0:1]), reads=[rq_, r_const, r_o], writes=[r_o])
                P.op("dve", lambda e: e.reciprocal(out=osq[:, 0:N], in_=osq[:, 0:N]), reads=[r_o], writes=[r_o])
                P.op("dve", lambda e: e.tensor_tensor(out=osb_[:, 0:N], in0=osb_[:, 0:N], in1=osq[:, 0:N], op=ALU.mult), reads=[r_o], writes=[r_o])
                P.op("dve", lambda e: e.tensor_tensor(out=yb[:, 0:N], in0=osb_[:, 0:N], in1=sgT[:, t0:t0 + N], op=ALU.mult), reads=[r_o, r_sg], writes=[r_yb])
                dma("sp", yT[12 + h, :, t0:t0 + N], yb[:, 0:N], reads=[r_yb], writes=[rD["yT"]])
        P.barrier()
        P.flush()
        es.close()

    for (st, l) in stages:
        if st == "A":
            stage_A(l)
        elif st == "X":
            stage_X(l)
        elif st == "B":
            stage_B(l, l == DEPTH - 1)
        elif st == "R":
            stage_R(l, l == DEPTH - 1)
        else:
            stage_C(l, l == DEPTH - 1)
    fin = list(cx.out_tickets)
    print("program ops:", P.nops, "sems:", len(P.semkeys), flush=True)
    P.barrier()
    t = P.op("sp", lambda e: e.dma_start(out=ident[:], in_=W["ident"][:, :]), writes=[r_const])
    P.flush([t] + fin)
    P.es.close()
    es_glob.close()
    return nc


def rope_tables(r):
    pos = np.arange(r * T, (r + 1) * T)
    row = (pos // 64).astype(np.float32)
    col = (pos % 64).astype(np.float32)

    def tab(rot_dim):
        nf = rot_dim // 4
        freqs = (ROPE_BASE ** (-np.arange(nf, dtype=np.float32) / nf)).astype(np.float32)
        ang = np.concatenate([row[:, None] * freqs, col[:, None] * freqs], -1)
        c = np.repeat(np.cos(ang), 2, axis=1).T
        s_ = np.repeat(np.sin(ang), 2, axis=1).T
        return c.astype(np.float32), s_.astype(np.float32)
    cA, sA = tab(32)
    cR, sR = tab(64)

    def withctx(c, s_):
        c = np.concatenate([c, np.ones((c.shape[0], CT), np.float32)], 1)
        s_ = np.concatenate([s_, np.zeros((s_.shape[0], CT), np.float32)], 1)
        return np.stack([c, s_], 0)
    ropeA = withctx(np.tile(cA, (4, 1)), np.tile(sA, (4, 1)))
    cM = np.concatenate([np.ones((64, T), np.float32), cA], 0)
    sM = np.concatenate([np.zeros((64, T), np.float32), sA], 0)
    ropeM = withctx(cM, sM)
    ropeR = withctx(cR, sR)
    return ropeA, ropeM, ropeR


def host_weights(inp, core):
    b, r = core // 4, core % 4
    f = np.float32
    w = {}
    cv = np.stack([inp["c"][b].reshape(8, 128).T, inp["c_ctx"].reshape(8, 128).T], -1)
    w["cvec"] = np.ascontiguousarray(cv, f)
    w["w_ada"] = inp["w_ada"]
    w["b_adaT"] = np.ascontiguousarray(inp["b_ada"].reshape(DEPTH, 48, 128).transpose(0, 2, 1))
    w["w_in"] = inp["w_in"]
    w["da_lambda"] = np.ascontiguousarray(inp["da_lambda"].reshape(DEPTH, 128))
    w["da_sublnT"] = np.ascontiguousarray(inp["da_subln"].reshape(DEPTH, 64, 1))
    w["gqT"] = np.ascontiguousarray(inp["mla_q_norm"].reshape(DEPTH, 2, 128).transpose(0, 2, 1))
    w["w_uq"] = inp["mla_w_uq"]
    w["gkvT"] = np.ascontiguousarray(inp["mla_kv_norm"].reshape(DEPTH, 128, 1))
    wk = inp["mla_w_ukv"].reshape(DEPTH, 128, 6, 128)
    w["w_ukv"] = np.ascontiguousarray(np.concatenate([wk[..., :64].reshape(DEPTH, 128, 384), wk[..., 64:].reshape(DEPTH, 128, 384)], -1))
    w["decay"] = np.ascontiguousarray(np.concatenate([inp["ret_decay_f"], inp["ret_decay_b"]], -1))
    w["w_out"] = inp["w_out"]
    for nm in ("ln1_g", "ln1_b", "ln2_g", "ln2_b"):
        w[nm + "T"] = np.ascontiguousarray(inp[nm].reshape(DEPTH, 8, 128).transpose(0, 2, 1))
    w["router_w"] = inp["router_w"]
    w["router_b"] = inp["router_b"]
    w["exp_w1"], w["exp_w3"], w["exp_w2"] = inp["exp_w1"], inp["exp_w3"], inp["exp_w2"]
    w["ropeA"], w["ropeM"], w["ropeR"] = rope_tables(r)
    w["tpos"] = np.ascontiguousarray((np.arange(32)[None, :] * 128 + np.arange(128)[:, None]).astype(f))
    BIG = 1.0e9
    eb = np.full((128, 16), BIG, f)
    for rp in range(4):
        if rp < r:
            eb[:, rp] = T * (r - 1 - rp)
        if rp > r:
            eb[:, 4 + rp] = T * (rp - r - 1)
    eb[:, 8] = T * r
    eb[:, 9] = T * (3 - r)
    w["ebound"] = eb
    w["ident"] = np.eye(128, dtype=f)
    mm = np.arange(128)[:, None].astype(f)
    ccc = np.arange(128)[None, :].astype(f)
    w["rtabs"] = np.ascontiguousarray(np.stack([np.maximum(ccc - mm, 0), np.maximum(mm - ccc, 0),
                                                 np.broadcast_to(ccc + 1, (128, 128)), np.broadcast_to(128 - ccc, (128, 128))], 0).astype(f))
    return w


PERLAYER_NAMES = ("w_ada", "b_adaT", "w_in", "da_lambda", "da_sublnT", "gqT", "w_uq", "gkvT", "w_ukv", "decay", "w_out",
                  "ln1_gT", "ln1_bT", "ln2_gT", "ln2_bT", "exp_w1", "exp_w3", "exp_w2")
STAGE_WEIGHTS = {
    "A": ("cvec", "w_ada", "b_adaT", "w_in", "gqT", "w_uq", "gkvT", "w_ukv", "decay", "ropeA", "ropeM", "ropeR", "tpos", "ident"),
    "X": ("ident",),
    "B": ("da_lambda", "da_sublnT", "ident"),
    "R": ("decay", "ebound", "tpos", "ident", "rtabs"),
    "C": ("cvec", "w_ada", "b_adaT", "w_out", "ln1_gT", "ln1_bT", "ln2_gT", "ln2_bT", "router_w", "router_b",
          "exp_w1", "exp_w3", "exp_w2", "ident"),
}
def stage_io(st, l):
    def xn(l_):
        if l_ == 0:
            return ["xT", "cxT"]
        if l_ == DEPTH:
            return ["xT_out"]
        return ["xTL%d" % l_, "cxTL%d" % l_]
    own = ["kda_own", "vda_own", "kml_own", "vml_own", "rsum_own"]
    gat = ["kda_all", "vda_all", "kml_all", "vml_all", "rsum_all"]
    if st == "A":
        return xn(l), ["qda", "qdaf", "qml", "qmlf", "qr", "kr", "krt", "vr", "sg"] + own
    if st == "X":
        return own, gat
    if st == "B":
        return ["qda", "qdaf", "kda_own", "vda_own", "qml", "qmlf", "kml_own", "vml_own", "kda_all", "vda_all", "kml_all", "vml_all"], ["yT"]
    if st == "R":
        return ["qr", "kr", "krt", "vr", "sg", "rsum_all", "yT"], ["yT"]
    return ["yT"] + xn(l), xn(l + 1)


NPDT = {"bf16": NPBF, "f32": np.float32}


def run_launch(stages, state, hw, keep=None):
    layers = sorted({l for _, l in stages})
    layer_of = {l: i for i, l in enumerate(layers)}
    wnames = []
    for st, _ in stages:
        for n in STAGE_WEIGHTS[st]:
            if n not in wnames:
                wnames.append(n)
    produced, ext_in, ext_out = set(), [], []
    for st, l_ in stages:
        rd, wr = stage_io(st, l_)
        for n in rd:
            if n not in produced and n not in ext_in:
                ext_in.append(n)
        for n in wr:
            produced.add(n)
            if n not in ext_out and n not in ext_in:
                ext_out.append(n)
    if keep is not None:
        ext_out = [n for n in ext_out if n in keep]
    in_maps = []
    wshapes = None
    for c in range(NCORE):
        m = {}
        for n in wnames:
            a = hw[c][n]
            if n in PERLAYER_NAMES:
                a = np.ascontiguousarray(a[layers])
            m[n] = a
        if wshapes is None:
            wshapes = {n: m[n].shape for n in wnames}
        for n in ext_in:
            m[n] = state[c][n]
        in_maps.append(m)
    nc = build_program(stages, layer_of, set(ext_in), set(ext_out), wshapes)
    res = run_bass_kernel_spmd(nc, in_maps, core_ids=list(range(NCORE)))
    for c in range(NCORE):
        for n in ext_out:
            state[c][n] = res.results[c][n]
    return res


FUSED = True


def kernel(**inputs):
    inp = {k: np.asarray(v) for k, v in inputs.items()}
    hw = [host_weights(inp, c) for c in range(NCORE)]
    state = []
    for c in range(NCORE):
        b, r = c // 4, c % 4
        xs = inp["x"][b, r * T:(r + 1) * T, :]
        state.append({"xT": np.ascontiguousarray(xs.T.reshape(8, 128, T)),
                      "cxT": np.ascontiguousarray(inp["ctx"][b].T.reshape(8, 128, CT))})
    allst = [(st, l) for l in range(DEPTH) for st in ("A", "X", "B", "R", "C")]
    if FUSED:
        run_launch(allst, state, hw, keep=("xT_out",))
    else:
        for l in range(DEPTH):
            run_launch([(st, l) for st in ("A", "X", "B", "R", "C")], state, hw,
                       keep=("xT_out", "xTL%d" % (l + 1), "cxTL%d" % (l + 1)))
    out = np.empty((2, S, D), np.float32)
    for c in range(NCORE):
        b, r = c // 4, c % 4
        out[b, r * T:(r + 1) * T, :] = state[c]["xT_out"].reshape(D, T).T
    return out
```

```python
import math
from contextlib import ExitStack
import numpy as np
import ml_dtypes
import concourse.bass as bass
import concourse.mybir as mybir
from concourse.bass_utils import run_bass_kernel_spmd

F32 = mybir.dt.float32
BF16 = mybir.dt.bfloat16
AF = mybir.ActivationFunctionType
ALU = mybir.AluOpType
AX = mybir.AxisListType
NPBF = ml_dtypes.bfloat16

D = 1024
S = 16384
T = 4096
CT = 256
NCORE = 8
DEPTH = 4
NE = 16
ALPHA = (2 * DEPTH) ** 0.25
LN_EPS = 1e-5
RMS_EPS = 1e-6
ROPE_BASE = 10000.0
EPOCH = 30000
CCDMA = 'cc'
DEBUG = False


class Res:
    __slots__ = ("name", "w", "r")

    def __init__(self, name=""):
        self.name = name
        self.w = None
        self.r = {}


class _Rec:
    def __init__(self):
        self.call = None

    def __getattr__(self, name):
        def f(*a, **k):
            self.call = (name, a, k)
            return self
        return f


class Prog:
    STREAMS = ("sp", "pe", "act", "dve", "pool")
    NSLOT = 8

    def __init__(self, nc):
        self.nc = nc
        self.ops = {e: [] for e in self.STREAMS}
        self.cnt = {e: 0 for e in self.STREAMS}
        self.known = {e: {} for e in self.STREAMS}
        self.semkeys = []
        self.semset = set()
        self.dma_slot = {q: 0 for q in self.STREAMS}
        self.dma_val = {}
        self.nops = 0
        self.pending = {e: None for e in self.STREAMS}
        self.sems = {}
        self.es = ExitStack()

    def _sem(self, key):
        if key not in self.semset:
            self.semset.add(key)
            self.semkeys.append(key)
        return key

    def op(self, eng, fn, reads=(), writes=(), dma=None):
        deps = {}

        def add(t):
            if t is None:
                return
            k, v = t
            if deps.get(k, -1) < v:
                deps[k] = v
        for r in reads:
            add(r.w)
        for w in writes:
            add(w.w)
            for t in w.r.items():
                add(t)
        if self.pending[eng]:
            for k, v in self.pending[eng].items():
                if deps.get(k, -1) < v:
                    deps[k] = v
            self.pending[eng] = None
        waits = []
        kn = self.known[eng]
        is_dma = (eng == "sp") if dma is None else dma
        if is_dma == "cc":
            key = self._sem(("cc", eng))
            val = self.dma_val.get(("cc", eng), 0) + 1
            self.dma_val[("cc", eng)] = val
            ticket = (key, val)
            inc = 1
        elif is_dma:
            slot = self.dma_slot[eng]
            self.dma_slot[eng] = (slot + 1) % self.NSLOT
            key = self._sem(("dma", eng, slot))
            prev = self.dma_val.get((eng, slot), 0)
            if prev:
                deps[key] = max(deps.get(key, 0), prev)
            val = prev + 16
            self.dma_val[(eng, slot)] = val
            ticket = (key, val)
            inc = 16
        else:
            n = self.cnt[eng]
            self.cnt[eng] = n + 1
            key = self._sem((eng, n // EPOCH))
            ticket = (key, n % EPOCH + 1)
            inc = 1
        for k, v in deps.items():
            if eng == "pe" and k[0] == "pe":
                continue
            if kn.get(k, 0) >= v:
                continue
            kn[k] = v
            waits.append((k, v))
        rec = _Rec()
        fn(rec)
        self.ops[eng].append((rec.call, waits, key, inc))
        for r in reads:
            if r.r.get(ticket[0], -1) < ticket[1]:
                r.r[ticket[0]] = ticket[1]
        for w in writes:
            w.w = ticket
            w.r = {}
        self.nops += 1
        return ticket

    def barrier(self):
        allk = {}
        for e in self.STREAMS:
            n = self.cnt[e]
            if n:
                allk[(e, (n - 1) // EPOCH)] = (n - 1) % EPOCH + 1
        for (q, slot), v in self.dma_val.items():
            allk[("cc", slot) if q == "cc" else ("dma", q, slot)] = v
        for e in self.STREAMS:
            self.pending[e] = dict(allk)

    def flush(self, final_tickets=()):
        nc = self.nc
        for k in self.semkeys:
            if k not in self.sems:
                self.sems[k] = self.es.enter_context(nc.semaphore("s%d" % len(self.sems)))
        sems = self.sems
        ops = self.ops
        self.ops = {e: [] for e in self.STREAMS}
        with nc.Block() as block:
            def replay(eh, oplist, tail=()):
                for (name, a, kw), waits, key, inc in oplist:
                    for k, v in waits:
                        eh.wait_ge(sems[k], v)
                    getattr(eh, name)(*a, **kw).then_inc(sems[key], inc)
                for k, v in tail:
                    eh.wait_ge(sems[k], v)

            @block.sync
            def _(e):
                replay(e, ops["sp"], tail=final_tickets)

            @block.tensor
            def _(e):
                replay(e, ops["pe"])

            @block.scalar
            def _(e):
                replay(e, ops["act"])

            @block.vector
            def _(e):
                replay(e, ops["dve"])

            @block.gpsimd
            def _(e):
                replay(e, ops["pool"])


def dram_specs():
    sp = {}
    b16, f32 = "bf16", "f32"
    TTL = T + CT
    sp["xT"] = ([8, 128, T], f32)
    sp["cxT"] = ([8, 128, CT], f32)
    for nm in ("qda", "qdaf", "kda_own"):
        sp[nm] = ([384, TTL], b16)
    sp["vda_own"] = ([6 * TTL, 65], b16)
    for nm in ("qml", "qmlf"):
        sp[nm] = ([6, 96, TTL], b16)
    sp["kml_own"] = ([576, TTL], b16)
    sp["vml_own"] = ([6 * TTL, 65], b16)
    for nm in ("qr", "kr", "sg"):
        sp[nm] = ([4, 64, TTL], b16)
    sp["krt"] = ([TTL, 256], b16)
    sp["vr"] = ([TTL, 256], b16)
    sp["rsum_own"] = ([64, 512], f32)
    sp["kda_all"] = ([12 * 4 * 32, TTL], b16)
    sp["vda_all"] = ([6 * 4 * TTL, 65], b16)
    sp["kml_all"] = ([12 * 4 * 48, TTL], b16)
    sp["vml_all"] = ([4 * 6 * TTL, 65], b16)
    sp["rsum_all"] = ([4 * 64, 512], f32)
    for l_ in range(1, DEPTH):
        sp["xTL%d" % l_] = ([8, 128, T], f32)
        sp["cxTL%d" % l_] = ([8, 128, CT], f32)
    sp["xT_out"] = ([8, 128, T], f32)
    sp["yT"] = ([16, 64, TTL], b16)
    sp["xT_next"] = ([8, 128, T], f32)
    sp["cxT_next"] = ([8, 128, CT], f32)
    sp["x1T"] = ([8, 128, TTL], f32)
    return sp


WEIGHT_SPECS = {
    "cvec": [128, 8, 2], "w_ada": [D, 6 * D], "b_adaT": [128, 48], "w_in": [D, 2592],
    "da_lambda": [128], "da_sublnT": [64, 1], "gqT": [128, 2], "w_uq": [256, 576], "gkvT": [128, 1],
    "w_ukv": [128, 768], "decay": [8], "w_out": [D, D], "ln1_gT": [128, 8], "ln1_bT": [128, 8],
    "ln2_gT": [128, 8], "ln2_bT": [128, 8], "router_w": [D, NE], "router_b": [NE],
    "exp_w1": [NE, D, 512], "exp_w3": [NE, D, 512], "exp_w2": [NE, 512, D],
    "ropeA": [2, 128, T], "ropeM": [2, 96, T], "ropeR": [2, 64, T], "tpos": [128, 32],
    "ebound": [128, 16], "ident": [128, 128], "maskpos": [128, 128],
}

TT = T + CT
GROUPS = [(g * 512, 512) for g in range(8)] + [(T, CT)]


class Ctx:
    def __init__(self, ext_in, ext_out):
        self.nc = bass.Bass("TRN2", target_bir_lowering=False)
        self.P = Prog(self.nc)
        self.dr = {}
        self.res = {}
        self.ext_in = ext_in
        self.ext_out = ext_out
        self.out_tickets = []

    def dram(self, name, shape=None, dt=None):
        if name in self.dr:
            return self.dr[name]
        if name in self.ext_in:
            kind = "ExternalInput"
        elif name in self.ext_out:
            kind = "ExternalOutput"
        else:
            kind = "Internal"
        t = self.nc.dram_tensor(name, list(shape), dt, kind=kind).ap()
        self.dr[name] = t
        self.res[name] = Res(name)
        return t


def build_program(stages, layer_of, ext_in, ext_out, wshapes):
    cx = Ctx(ext_in, ext_out)
    nc, P = cx.nc, cx.P
    specs = dram_specs()
    DT = {"bf16": BF16, "f32": F32}

    def dget(name):
        shp, dt = specs[name]
        return cx.dram(name, shp, DT[dt])

    def xnames(l):
        if l == 0:
            return "xT", "cxT"
        if l == DEPTH:
            return "xT_out", None
        return "xTL%d" % l, "cxTL%d" % l

    class WL:
        def __init__(self, ap):
            self.ap = ap

        def __getitem__(self, key):
            if isinstance(key, tuple):
                return self.ap[(layer_of[key[0]],) + tuple(key[1:])]
            return self.ap[layer_of[key]]
    W = {}
    PERLAYER = ("w_ada", "b_adaT", "w_in", "da_lambda", "da_sublnT", "gqT", "w_uq", "gkvT", "w_ukv", "decay", "w_out",
                "ln1_gT", "ln1_bT", "ln2_gT", "ln2_bT", "exp_w1", "exp_w3", "exp_w2")
    for nm, shp in wshapes.items():
        t = nc.dram_tensor(nm, list(shp), F32, kind="ExternalInput").ap()
        W[nm] = WL(t) if nm in PERLAYER else t

    es_glob = ExitStack()
    ps_all = es_glob.enter_context(nc.psum_tensor("ps_all", [128, 4096], F32))
    psb = [ps_all[:, i * 512:(i + 1) * 512] for i in range(8)]
    psbf = ps_all[:, 3584:4096].bitcast(BF16)
    psr = [Res("ps%d" % i) for i in range(8)]
    psbf_r = psr[7]
    rot = {"i": 0}

    def next_ps(lo=0, hi=6):
        i = lo + rot["i"] % (hi - lo)
        rot["i"] += 1
        return psb[i], psr[i]

    def dma(q, out, in_, reads=(), writes=()):
        return P.op(q, lambda e: e.dma_start(out=out, in_=in_), reads, writes, dma=True)

    ucnt = {"n": 0}

    def sbuf(es, name, shape, dt):
        ucnt["n"] += 1
        return es.enter_context(nc.sbuf_tensor("sb%d_%s" % (ucnt["n"], name), list(shape), dt))

    ident = sbuf(es_glob, "ident", [128, 128], F32)
    identb = sbuf(es_glob, "identb", [128, 128], BF16)
    ones = sbuf(es_glob, "ones", [128, 128], F32)
    epsr = sbuf(es_glob, "epsr", [128, 2], F32)
    r_const = Res("const")
    dma("sp", ident[:], W["ident"][:, :], writes=[r_const])
    P.op("dve", lambda e: e.tensor_copy(out=identb[:], in_=ident[:]), reads=[r_const], writes=[r_const])
    P.op("dve", lambda e: e.memset(ones[:], 1.0), writes=[r_const])
    P.op("dve", lambda e: e.memset(epsr[:, 0:1], RMS_EPS), writes=[r_const])
    P.op("dve", lambda e: e.memset(epsr[:, 1:2], LN_EPS), writes=[r_const])

    mod_cache = {}

    def compute_mod(es, l):
        if l in mod_cache:
            return mod_cache[l]
        modT = sbuf(es_glob, "modT%d" % l, [128, 48, 2], F32)
        r_mod = Res("mod")
        mod_cache[l] = (modT, r_mod)
        with ExitStack() as e2:
            sc = sbuf(e2, "sc", [128, 8, 2], F32)
            bad = sbuf(e2, "bad", [128, 48], F32)
            stg = [sbuf(e2, "stgA%d" % i, [128, 8, 512], F32) for i in range(2)]
            r_sc, r_bad = Res(), Res()
            r_stg = [Res(), Res()]
            dma("sp", sc[:], W["cvec"][:, :, :], writes=[r_sc])
            dma("sp", bad[:], W["b_adaT"][l], writes=[r_bad])
            P.op("act", lambda e: e.activation(out=sc[:], in_=sc[:], func=AF.Silu), reads=[r_sc], writes=[r_sc])
            pm, pmr = psb[6], psr[6]
            wv = W["w_ada"][l].rearrange("(k p) n -> p k n", p=128)
            for jb in range(12):
                st, rs = stg[jb % 2], r_stg[jb % 2]
                dma("sp", st[:], wv[:, :, jb * 512:(jb + 1) * 512], writes=[rs])
                for j in range(4):
                    col = (jb * 4 + j) * 2
                    for k in range(8):
                        P.op("pe", lambda e, st=st, j=j, k=k, col=col: e.matmul(
                            pm[:, col:col + 2], lhsT=st[:, k, j * 128:(j + 1) * 128], rhs=sc[:, k, :],
                            start=(k == 0), stop=(k == 7)), reads=[rs, r_sc], writes=[pmr])
            for i in range(2):
                P.op("dve", lambda e, i=i: e.tensor_tensor(out=modT[:, :, i], in0=pm[:, i:96:2], in1=bad[:],
                                                          op=ALU.add), reads=[pmr, r_bad], writes=[r_mod])
            for m in (1, 4):
                P.op("dve", lambda e, m=m: e.tensor_scalar_add(out=modT[:, m * 8:(m + 1) * 8, :],
                                                              in0=modT[:, m * 8:(m + 1) * 8, :], scalar1=1.0),
                     reads=[r_mod], writes=[r_mod])
            P.barrier()
        return modT, r_mod

    def load_lg(es, l):
        lg = sbuf(es, "lg", [128, 8], F32)
        r_lg = Res("lg")
        dma("sp", lg[:], W["decay"][l].partition_broadcast(128), writes=[r_lg])
        P.op("act", lambda e: e.activation(out=lg[:], in_=lg[:], func=AF.Exp, scale=-1.0), reads=[r_lg], writes=[r_lg])
        P.op("act", lambda e: e.activation(out=lg[:], in_=lg[:], func=AF.Ln, bias=ones[:, 0:1], scale=1.0),
             reads=[r_lg, r_const], writes=[r_lg])
        P.op("dve", lambda e: e.tensor_scalar_mul(out=lg[:], in0=lg[:], scalar1=-1.0), reads=[r_lg], writes=[r_lg])
        return lg, r_lg

    def stage_A(l):
        es = ExitStack()
        modT, r_mod = compute_mod(es, l)
        lg, r_lg = load_lg(es, l)
        xn, cxn = xnames(l)
        xT, cxT = dget(xn), dget(cxn)
        win = sbuf(es, "win", [128, 8, 2592], BF16)
        wsw = sbuf(es, "wsw", [128, 8, 1312], BF16)
        wuq = sbuf(es, "wuq", [128, 2, 576], BF16)
        wuqs = sbuf(es, "wuqs", [128, 2, 576], BF16)
        wukv = sbuf(es, "wukv", [128, 768], BF16)
        gq = sbuf(es, "gq", [128, 3], F32)
        r_w = Res("w")
        dma("sp", gq[:, 0:2], W["gqT"][l], writes=[r_w])
        dma("sp", gq[:, 2:3], W["gkvT"][l], writes=[r_w])
        with ExitStack() as e2:
            stg = [sbuf(e2, "stgw%d" % i, [128, 2592], F32) for i in range(2)]
            r_stg = [Res(), Res()]
            for k in range(8):
                st, rs = stg[k % 2], r_stg[k % 2]
                dma("sp", st[:], W["w_in"][l, k * 128:(k + 1) * 128, :], writes=[rs])
                P.op("dve", lambda e, st=st: e.tensor_scalar_mul(out=st[:, 1824:2080], in0=st[:, 1824:2080], scalar1=0.125),
                     reads=[rs], writes=[rs])
                P.op("dve", lambda e, st=st, k=k: e.tensor_copy(out=win[:, k, :], in_=st[:]), reads=[rs], writes=[r_w])
                for (s0, n, d0) in ((0, 768, 0), (1536, 544, 768)):
                    P.op("pool", lambda e, st=st, k=k, s0=s0, n=n, d0=d0: e.tensor_scalar_mul(
                        out=wsw[:, k, d0:d0 + n:2], in0=st[:, s0 + 1:s0 + n:2], scalar1=-1.0), reads=[rs], writes=[r_w])
                    P.op("pool", lambda e, st=st, k=k, s0=s0, n=n, d0=d0: e.tensor_copy(
                        out=wsw[:, k, d0 + 1:d0 + n:2], in_=st[:, s0:s0 + n:2]), reads=[rs], writes=[r_w])
            st = stg[0]
            stv = st[:, 0:1152].rearrange("p (k n) -> p k n", k=2)
            dma("sp", stv, W["w_uq"][l].rearrange("(k p) n -> p k n", p=128), writes=[r_stg[0]])
            P.op("pool", lambda e: e.memset(wuqs[:], 0.0), writes=[r_w])
            for k in range(2):
                P.op("dve", lambda e, k=k: e.tensor_scalar_mul(out=stv[:, k, :], in0=stv[:, k, :], scalar1=gq[:, k:k + 1]),
                     reads=[r_stg[0], r_w], writes=[r_stg[0]])
                P.op("dve", lambda e, k=k: e.tensor_copy(out=wuq[:, k, :], in_=stv[:, k, :]), reads=[r_stg[0]], writes=[r_w])
                sv = stv[:, k, :].rearrange("p (h d) -> p h d", h=6)
                dv = wuqs[:, k, :].rearrange("p (h d) -> p h d", h=6)
                P.op("pool", lambda e, sv=sv, dv=dv: e.tensor_scalar_mul(out=dv[:, :, 64:96:2], in0=sv[:, :, 65:96:2], scalar1=-1.0),
                     reads=[r_stg[0]], writes=[r_w])
                P.op("pool", lambda e, sv=sv, dv=dv: e.tensor_copy(out=dv[:, :, 65:96:2], in_=sv[:, :, 64:96:2]),
                     reads=[r_stg[0]], writes=[r_w])
            st1 = stg[1]
            dma("sp", st1[:, 0:768], W["w_ukv"][l], writes=[r_stg[1]])
            P.op("dve", lambda e: e.tensor_scalar_mul(out=wukv[:], in0=st1[:, 0:768], scalar1=gq[:, 2:3]),
                 reads=[r_stg[1], r_w], writes=[r_w])
            P.barrier()

        tpos = sbuf(es, "tpos", [128, 32], F32)
        epos = sbuf(es, "epos", [128, 32], F32)
        wfb = sbuf(es, "wfb", [128, 32, 8], F32)
        r_wfb = Res("wfb")
        dma("sp", tpos[:], W["tpos"][:, :], writes=[r_wfb])
        P.op("dve", lambda e: e.tensor_scalar(out=epos[:], in0=tpos[:], scalar1=-1.0, scalar2=float(T - 1),
                                              op0=ALU.mult, op1=ALU.add), reads=[r_wfb], writes=[r_wfb])
        for h in range(4):
            P.op("act", lambda e, h=h: e.activation(out=wfb[:, :, h], in_=epos[:], func=AF.Exp, scale=lg[:, h:h + 1]),
                 reads=[r_wfb, r_lg], writes=[r_wfb])
            P.op("act", lambda e, h=h: e.activation(out=wfb[:, :, 4 + h], in_=tpos[:], func=AF.Exp, scale=lg[:, 4 + h:5 + h]),
                 reads=[r_wfb, r_lg], writes=[r_wfb])

        NB = 2
        xg = [sbuf(es, "xg%d" % i, [128, 8, 512], F32) for i in range(NB)]
        hT = [sbuf(es, "hT%d" % i, [128, 8, 512], BF16) for i in range(NB)]
        r_xg = [Res() for _ in range(NB)]
        r_hT = [Res() for _ in range(NB)]
        tabA = [sbuf(es, "tabA%d" % i, [128, 2, 512], F32) for i in range(NB)]
        tabM = [sbuf(es, "tabM%d" % i, [96, 2, 512], F32) for i in range(NB)]
        tabR = [sbuf(es, "tabR%d" % i, [64, 2, 512], F32) for i in range(NB)]
        r_tab = [Res() for _ in range(NB)]
        NTMP = 3
        tmp1 = [sbuf(es, "tmp1_%d" % i, [128, 512], F32) for i in range(NTMP)]
        tmp2 = [sbuf(es, "tmp2_%d" % i, [128, 512], F32) for i in range(NTMP)]
        r_tmp = [Res() for _ in range(NTMP)]
        NOB = 4
        ob = [sbuf(es, "ob%d" % i, [128, 512], BF16) for i in range(NOB)]
        ob2 = [sbuf(es, "ob2_%d" % i, [128, 512], BF16) for i in range(NOB)]
        r_ob = [Res() for _ in range(NOB)]
        r_ob2 = [Res() for _ in range(NOB)]
        cqn = sbuf(es, "cqn", [128, 2, 512], BF16)
        ckvn = sbuf(es, "ckvn", [128, 512], BF16)
        sq = sbuf(es, "sq", [128, 3, 512], F32)
        rstd = sbuf(es, "rstd", [128, 2, 512], F32)
        r_cqn, r_ckvn, r_sq, r_rstd = Res(), Res(), Res(), Res()
        vt = [sbuf(es, "vt%d" % i, [128, 6, 65], BF16) for i in range(4)]
        r_vt = [Res() for _ in range(4)]
        for i in range(4):
            P.op("pool", lambda e, i=i: e.memset(vt[i][:], 1.0), writes=[r_vt[i]])
        vrt = [sbuf(es, "vrt%d" % i, [128, 256], BF16) for i in range(2)]
        krt_t = [sbuf(es, "krt_t%d" % i, [128, 256], BF16) for i in range(2)]
        kw = [sbuf(es, "kw%d" % i, [128, 8, 64], BF16) for i in range(2)]
        r_vrt = [Res(), Res()]
        r_krt = [Res(), Res()]
        r_kw = [Res(), Res()]
        krg = sbuf(es, "krg", [64, 4, 512], BF16)
        r_krg = Res()
        cnt = {"tmp": 0, "ob": 0, "vt": 0}
        psU, rU = psb[6], psr[6]

        D_ = {n: dget(n) for n in ("qda", "qdaf", "kda_own", "vda_own", "qml", "qmlf", "kml_own", "vml_own",
                                    "qr", "kr", "krt", "vr", "sg", "rsum_own")}
        rD = cx.res

        def proj(M, N, terms, reads):
            ps, pr = next_ps()
            n = len(terms)
            for i, (lh, rh) in enumerate(terms):
                P.op("pe", lambda e, lh=lh, rh=rh, i=i: e.matmul(ps[0:M, 0:N], lhsT=lh, rhs=rh, start=(i == 0), stop=(i == n - 1)),
                     reads=reads, writes=[pr])
            return ps, pr

        def rope(M, N, pa, ra, pb, rb, tab, rt, rows0=0):
            i = cnt["tmp"] % NTMP
            cnt["tmp"] += 1
            j = cnt["ob"] % NOB
            cnt["ob"] += 1
            P.op("dve", lambda e: e.tensor_tensor(out=tmp1[i][0:M, 0:N], in0=pa[0:M, 0:N], in1=tab[rows0:rows0 + M, 0, 0:N], op=ALU.mult),
                 reads=[ra, rt], writes=[r_tmp[i]])
            P.op("dve", lambda e: e.tensor_tensor(out=tmp2[i][0:M, 0:N], in0=pb[0:M, 0:N], in1=tab[rows0:rows0 + M, 1, 0:N], op=ALU.mult),
                 reads=[rb, rt], writes=[r_tmp[i]])
            P.op("pool", lambda e: e.tensor_tensor(out=ob[j][0:M, 0:N], in0=tmp1[i][0:M, 0:N], in1=tmp2[i][0:M, 0:N], op=ALU.add),
                 reads=[r_tmp[i]], writes=[r_ob[j]])
            return ob[j], r_ob[j]

        def plain(M, N, pa, ra, func=AF.Copy):
            j = cnt["ob"] % NOB
            cnt["ob"] += 1
            P.op("act", lambda e: e.activation(out=ob2[j][0:M, 0:N], in_=pa[0:M, 0:N], func=func), reads=[ra], writes=[r_ob2[j]])
            return ob2[j], r_ob2[j]

        for gi, (t0, N) in enumerate(GROUPS):
            b = gi % NB
            isctx = (t0 == T)
            mi = 1 if isctx else 0
            src = cxT.rearrange("c p t -> p c t") if isctx else xT.rearrange("c p t -> p c t")[:, :, t0:t0 + N]
            dma("sp", xg[b][:, :, 0:N], src, reads=[rD[cxn if isctx else xn]], writes=[r_xg[b]])
            dma("sp", tabA[b][:, :, 0:N], W["ropeA"].rearrange("c p t -> p c t")[:, :, t0:t0 + N], writes=[r_tab[b]])
            dma("sp", tabM[b][:, :, 0:N], W["ropeM"].rearrange("c p t -> p c t")[:, :, t0:t0 + N], writes=[r_tab[b]])
            dma("sp", tabR[b][:, :, 0:N], W["ropeR"].rearrange("c p t -> p c t")[:, :, t0:t0 + N], writes=[r_tab[b]])
            for c in range(8):
                P.op("dve", lambda e, c=c: e.tensor_scalar(out=hT[b][:, c, 0:N], in0=xg[b][:, c, 0:N],
                                                          scalar1=modT[:, 8 + c, mi:mi + 1], scalar2=modT[:, c, mi:mi + 1],
                                                          op0=ALU.mult, op1=ALU.add), reads=[r_xg[b], r_mod], writes=[r_hT[b]])
            hb, rh = hT[b], r_hT[b]
            if gi == 0 and DEBUG:
                d1 = nc.dram_tensor("dbg_mod", [128, 96], F32, kind="ExternalOutput").ap()
                d2 = nc.dram_tensor("dbg_h", [128, 8, 512], BF16, kind="ExternalOutput").ap()
                d3 = nc.dram_tensor("dbg_win", [128, 8, 2592], BF16, kind="ExternalOutput").ap()
                dma("sp", d1[:, :], modT[:].rearrange("p a b -> p (a b)"), reads=[r_mod])
                dma("sp", d2[:, :, :], hb[:], reads=[rh])
                dma("sp", d3[:, :, :], win[:], reads=[r_w])

            def wterms(wt, c0, M):
                return [(wt[:, k, c0:c0 + M], hb[:, k, 0:N]) for k in range(8)]

            for c in range(3):
                pa, ra = proj(128, N, wterms(win, c * 128, 128), [rh, r_w])
                pb, rb = proj(128, N, wterms(wsw, c * 128, 128), [rh, r_w])
                o, ro = rope(128, N, pa, ra, pb, rb, tabA[b], r_tab[b])
                dma("act", D_["qda"][c * 128:(c + 1) * 128, t0:t0 + N], o[:, 0:N], reads=[ro], writes=[rD["qda"]])
                o2, ro2 = plain(128, N, pa, ra)
                dma("act", D_["qdaf"][c * 128:(c + 1) * 128, t0:t0 + N], o2[:, 0:N], reads=[ro2], writes=[rD["qdaf"]])
            for c in range(3):
                pa, ra = proj(128, N, wterms(win, 384 + c * 128, 128), [rh, r_w])
                pb, rb = proj(128, N, wterms(wsw, 384 + c * 128, 128), [rh, r_w])
                o, ro = rope(128, N, pa, ra, pb, rb, tabA[b], r_tab[b])
                dma("act", D_["kda_own"][c * 128:(c + 1) * 128, t0:t0 + N], o[:, 0:N], reads=[ro], writes=[rD["kda_own"]])
            pcq = [proj(128, N, wterms(win, 1152 + c * 128, 128), [rh, r_w]) for c in range(2)]
            pckv = proj(128, N, wterms(win, 1408, 128), [rh, r_w])
            for c, (pp, rr) in enumerate(pcq + [pckv]):
                P.op("act", lambda e, c=c, pp=pp: e.activation(out=sq[:, c, 0:N], in_=pp[:, 0:N], func=AF.Square), reads=[rr], writes=[r_sq])
            pbq, rbq = proj(128, N, [(ones[:, :], sq[:, c, 0:N]) for c in range(2)], [r_sq, r_const])
            pbk, rbk = proj(128, N, [(ones[:, :], sq[:, 2, 0:N])], [r_sq, r_const])
            P.op("act", lambda e: e.activation(out=rstd[:, 0, 0:N], in_=pbq[:, 0:N], func=AF.Sqrt, scale=1.0 / 256, bias=epsr[:, 0:1]),
                 reads=[rbq, r_const], writes=[r_rstd])
            P.op("act", lambda e: e.activation(out=rstd[:, 1, 0:N], in_=pbk[:, 0:N], func=AF.Sqrt, scale=1.0 / 128, bias=epsr[:, 0:1]),
                 reads=[rbk, r_const], writes=[r_rstd])
            P.op("dve", lambda e: e.reciprocal(out=rstd[:, :, 0:N], in_=rstd[:, :, 0:N]), reads=[r_rstd], writes=[r_rstd])
            for c in range(2):
                P.op("dve", lambda e, c=c: e.tensor_tensor(out=cqn[:, c, 0:N], in0=pcq[c][0][:, 0:N], in1=rstd[:, 0, 0:N], op=ALU.mult),
                     reads=[pcq[c][1], r_rstd], writes=[r_cqn])
            P.op("dve", lambda e: e.tensor_tensor(out=ckvn[:, 0:N], in0=pckv[0][:, 0:N], in1=rstd[:, 1, 0:N], op=ALU.mult),
                 reads=[pckv[1], r_rstd], writes=[r_ckvn])
            for h in range(6):
                pa, ra = proj(96, N, [(wuq[:, k, h * 96:(h + 1) * 96], cqn[:, k, 0:N]) for k in range(2)], [r_cqn, r_w])
                pb, rb = proj(96, N, [(wuqs[:, k, h * 96:(h + 1) * 96], cqn[:, k, 0:N]) for k in range(2)], [r_cqn, r_w])
                o, ro = rope(96, N, pa, ra, pb, rb, tabM[b], r_tab[b])
                dma("act", D_["qml"][h, :, t0:t0 + N], o[0:96, 0:N], reads=[ro], writes=[rD["qml"]])
                o2, ro2 = plain(96, N, pa, ra)
                dma("act", D_["qmlf"][h, :, t0:t0 + N], o2[0:96, 0:N], reads=[ro2], writes=[rD["qmlf"]])
            for c in range(3):
                pa, ra = proj(128, N, [(wukv[:, c * 128:(c + 1) * 128], ckvn[:, 0:N])], [r_ckvn, r_w])
                o2, ro2 = plain(128, N, pa, ra)
                for hh in range(2):
                    dma("act", D_["kml_own"][(2 * c + hh) * 96:(2 * c + hh) * 96 + 64, t0:t0 + N], o2[hh * 64:(hh + 1) * 64, 0:N], reads=[ro2],
                        writes=[rD["kml_own"]])
            pa, ra = proj(32, N, wterms(win, 1536, 32), [rh, r_w])
            pb, rb = proj(32, N, wterms(wsw, 768, 32), [rh, r_w])
            o, ro = rope(32, N, pa, ra, pb, rb, tabA[b], r_tab[b])
            for h in range(6):
                dma("act", D_["kml_own"][h * 96 + 64:h * 96 + 96, t0:t0 + N], o[0:32, 0:N], reads=[ro], writes=[rD["kml_own"]])
            for h in range(4):
                pa, ra = proj(64, N, wterms(win, 1568 + h * 64, 64), [rh, r_w])
                pb, rb = proj(64, N, wterms(wsw, 800 + h * 64, 64), [rh, r_w])
                o, ro = rope(64, N, pa, ra, pb, rb, tabR[b], r_tab[b])
                dma("act", D_["qr"][h, :, t0:t0 + N], o[0:64, 0:N], reads=[ro], writes=[rD["qr"]])
            for h in range(4):
                pa, ra = proj(64, N, wterms(win, 1824 + h * 64, 64), [rh, r_w])
                pb, rb = proj(64, N, wterms(wsw, 1056 + h * 64, 64), [rh, r_w])
                o, ro = rope(64, N, pa, ra, pb, rb, tabR[b], r_tab[b])
                dma("act", D_["kr"][h, :, t0:t0 + N], o[0:64, 0:N], reads=[ro], writes=[rD["kr"]])
                P.op("pool", lambda e, h=h, o=o: e.tensor_copy(out=krg[:, h, 0:N], in_=o[0:64, 0:N]), reads=[ro], writes=[r_krg])
            for h in range(4):
                pa, ra = proj(64, N, wterms(win, 2336 + h * 64, 64), [rh, r_w])
                o2, ro2 = plain(64, N, pa, ra, func=AF.Silu)
                dma("act", D_["sg"][h, :, t0:t0 + N], o2[0:64, 0:N], reads=[ro2], writes=[rD["sg"]])
            for j in range(N // 128):
                ts = slice(j * 128, (j + 1) * 128)
                tg = t0 + j * 128
                for (wt_terms, dname) in (([(hb[:, k, ts], win[:, k, 768:1152]) for k in range(8)], "vda_own"),
                                          ([(ckvn[:, ts], wukv[:, 384:768])], "vml_own")):
                    ps, pr = next_ps()
                    n = len(wt_terms)
                    for i, (lh, rhh) in enumerate(wt_terms):
                        P.op("pe", lambda e, ps=ps, lh=lh, rhh=rhh, i=i, n=n: e.matmul(ps[:, 0:384], lhsT=lh, rhs=rhh, start=(i == 0), stop=(i == n - 1)),
                             reads=[rh, r_w, r_ckvn], writes=[pr])
                    vi = cnt["vt"] % 4
                    cnt["vt"] += 1
                    P.op("dve", lambda e, ps=ps, vi=vi: e.tensor_copy(out=vt[vi][:, :, 0:64], in_=ps[:, 0:384].rearrange("p (h d) -> p h d", h=6)),
                         reads=[pr], writes=[r_vt[vi]])
                    dma("act", D_[dname].rearrange("(h t) e -> t h e", h=6)[tg:tg + 128, :, :], vt[vi][:], reads=[r_vt[vi]], writes=[rD[dname]])
                jj = j % 2
                ps, pr = next_ps()
                for k in range(8):
                    P.op("pe", lambda e, ps=ps, k=k: e.matmul(ps[:, 0:256], lhsT=hb[:, k, ts], rhs=win[:, k, 2080:2336], start=(k == 0), stop=(k == 7)),
                         reads=[rh, r_w], writes=[pr])
                P.op("dve", lambda e, ps=ps, jj=jj: e.tensor_copy(out=vrt[jj][:], in_=ps[:, 0:256]), reads=[pr], writes=[r_vrt[jj]])
                dma("act", D_["vr"][tg:tg + 128, :], vrt[jj][:], reads=[r_vrt[jj]], writes=[rD["vr"]])
                for h in range(4):
                    P.op("pe", lambda e, h=h: e.transpose(psbf[:, h * 64:(h + 1) * 64], krg[:, h, ts], identb[0:64, 0:64]),
                         reads=[r_krg, r_const], writes=[psbf_r])
                P.op("dve", lambda e, jj=jj: e.tensor_copy(out=krt_t[jj][:], in_=psbf[:, 0:256]), reads=[psbf_r], writes=[r_krt[jj]])
                dma("act", D_["krt"][tg:tg + 128, :], krt_t[jj][:], reads=[r_krt[jj]], writes=[rD["krt"]])
                if not isctx:
                    tile_i = tg // 128
                    for dd in range(2):
                        for h in range(4):
                            P.op("pool", lambda e, jj=jj, dd=dd, h=h, tile_i=tile_i: e.tensor_scalar_mul(
                                out=kw[jj][:, dd * 4 + h, :], in0=krt_t[jj][:, h * 64:(h + 1) * 64],
                                scalar1=wfb[:, tile_i, dd * 4 + h:dd * 4 + h + 1]), reads=[r_krt[jj], r_wfb], writes=[r_kw[jj]])
                    for dd in range(2):
                        for h in range(4):
                            col = (dd * 4 + h) * 64
                            P.op("pe", lambda e, jj=jj, dd=dd, h=h, col=col, tile_i=tile_i: e.matmul(
                                psU[0:64, col:col + 64], lhsT=kw[jj][:, dd * 4 + h, :], rhs=vrt[jj][:, h * 64:(h + 1) * 64],
                                start=(tile_i == 0 and dd == 0 and h == 0), stop=(tile_i == 31 and dd == 1 and h == 3)), reads=[r_kw[jj], r_vrt[jj]], writes=[rU])
        usb = sbuf(es, "usb", [64, 512], F32)
        r_usb = Res()
        P.op("dve", lambda e: e.tensor_copy(out=usb[:], in_=psU[0:64, :]), reads=[rU], writes=[r_usb])
        dma("act", D_["rsum_own"][:, :], usb[:], reads=[r_usb], writes=[rD["rsum_own"]])
        P.barrier()
        P.flush()
        es.close()

    def stage_X(l):
        RG = [[0, 1, 2, 3], [4, 5, 6, 7]]

        def ag(a, b_, r0, n, o0):
            src, dst = dget(a), dget(b_)
            P.op("pool", lambda e: e.collective_compute("AllGather", ALU.bypass, replica_groups=RG, ins=[src[r0:r0 + n, :]],
                                                        outs=[dst[o0:o0 + 4 * n, :]]),
                 reads=[cx.res[a]], writes=[cx.res[b_]], dma=CCDMA)
        for mi in range(12):
            ag("kda_own", "kda_all", mi * 32, 32, mi * 128)
        for h in range(6):
            ag("vda_own", "vda_all", h * TT, TT, h * 4 * TT)
        for ch in range(12):
            ag("kml_own", "kml_all", ch * 48, 48, ch * 192)
        for h in range(6):
            ag("vml_own", "vml_all", h * TT, TT, h * 4 * TT)
        ag("rsum_own", "rsum_all", 0, 64, 0)
        P.flush()

    def stage_B(l, last):
        es = ExitStack()
        yT = dget("yT")
        rY = cx.res["yT"]
        lam = sbuf(es, "lam", [128, 128], F32)
        lsc = sbuf(es, "lsc", [128, 4], F32)
        gsub = sbuf(es, "gsub", [64, 1], F32)
        r_l = Res()
        lam_init = 0.8 - 0.6 * math.exp(-0.3 * l)
        dma("sp", lam[:], W["da_lambda"][l].partition_broadcast(128), writes=[r_l])
        dma("sp", gsub[:], W["da_sublnT"][l], writes=[r_l])
        P.op("dve", lambda e: e.tensor_tensor(out=lam[:, 0:32], in0=lam[:, 0:32], in1=lam[:, 32:64], op=ALU.mult), reads=[r_l], writes=[r_l])
        P.op("dve", lambda e: e.tensor_tensor(out=lam[:, 64:96], in0=lam[:, 64:96], in1=lam[:, 96:128], op=ALU.mult), reads=[r_l], writes=[r_l])
        P.op("dve", lambda e: e.reduce_sum(out=lsc[:, 0:1], in_=lam[:, 0:32], axis=AX.X), reads=[r_l], writes=[r_l])
        P.op("dve", lambda e: e.reduce_sum(out=lsc[:, 1:2], in_=lam[:, 64:96], axis=AX.X), reads=[r_l], writes=[r_l])
        P.op("act", lambda e: e.activation(out=lsc[:, 0:2], in_=lsc[:, 0:2], func=AF.Exp), reads=[r_l], writes=[r_l])
        P.op("dve", lambda e: e.tensor_tensor(out=lsc[:, 2:3], in0=lsc[:, 0:1], in1=lsc[:, 1:2], op=ALU.subtract), reads=[r_l], writes=[r_l])
        P.op("dve", lambda e: e.tensor_scalar(out=lsc[:, 3:4], in0=lsc[:, 2:3], scalar1=lam_init, scalar2=-1.0, op0=ALU.add, op1=ALU.mult),
             reads=[r_l], writes=[r_l])
        P.op("dve", lambda e: e.tensor_scalar_mul(out=gsub[:], in0=gsub[:], scalar1=(1.0 - lam_init)), reads=[r_l], writes=[r_l])
        sel = sbuf(es, "sel", [65, 64], F32)
        P.op("dve", lambda e: e.memset(sel[:], 0.0), writes=[r_l])
        P.op("dve", lambda e: e.memset(sel[64:65, :], 1.0), writes=[r_l])

        KT = [sbuf(es, "KT%d" % i, [128, S + CT], BF16) for i in range(2)]
        VV = [sbuf(es, "VV%d" % i, [128, 130, 65], BF16) for i in range(2)]
        QT = [sbuf(es, "QT%d" % i, [128, 2, TT], BF16) for i in range(2)]
        r_KT = [Res(), Res()]
        r_VV = [Res(), Res()]
        r_QT = [Res(), Res()]
        NPT = 3
        PT = [sbuf(es, "PT%d" % i, [128, 512], BF16) for i in range(NPT)]
        r_PT = [Res() for _ in range(NPT)]
        osb = [sbuf(es, "osb%d" % i, [65, 512], F32) for i in range(2)]
        r_osb = [Res(), Res()]
        rec = [sbuf(es, "rec%d" % i, [64, 512], F32) for i in range(2)]
        r_rec = [Res(), Res()]
        od = sbuf(es, "od", [64, 512], F32)
        od2 = sbuf(es, "od2", [64, 512], F32)
        r_od = Res()
        yb = [sbuf(es, "yb%d" % i, [64, 512], BF16) for i in range(2)]
        r_yb = [Res(), Res()]
        ctr = {"pt": 0, "u": 0, "y": 0}
        kda_all, vda_all, kml_all, vml_all = dget("kda_all"), dget("vda_all"), dget("kml_all"), dget("vml_all")
        kda_own, vda_own, kml_own, vml_own = dget("kda_own"), dget("vda_own"), dget("kml_own"), dget("vml_own")
        qda, qdaf, qml, qmlf = dget("qda"), dget("qdaf"), dget("qml"), dget("qmlf")
        rD = cx.res

        SG = [ps_all[:, 0:1536], ps_all[:, 1536:3072]]
        r_SG = [Res(), Res()]
        PT3 = [sbuf(es, "PT3_%d" % i, [128, 3, 512], BF16) for i in range(2)]
        r_PT3 = [Res(), Res()]

        def attend(kt, rk, vv, rv, qt, rq, dk, scale, t0, N, isctx, ps_o, r_o):
            tiles = ([] if isctx else [(j, False) for j in range(128)]) + [(128, True), (129, True)]
            grp = [tiles[i:i + 3] for i in range(0, len(tiles), 3)]
            G = len(grp)
            nt = len(tiles)

            def qk(g):
                sg, rs = SG[g % 2], r_SG[g % 2]
                for k, (j, cx_) in enumerate(grp[g]):
                    pb = 32 * k if dk == 32 else 0
                    if cx_:
                        lh = kt[pb:pb + dk, S + (j - 128) * 128:S + (j - 127) * 128]
                        rh_ = qt[pb:pb + dk, 1, t0:t0 + N]
                    else:
                        lh = kt[pb:pb + dk, j:S:128]
                        rh_ = qt[pb:pb + dk, 0, t0:t0 + N]
                    P.op("pe", lambda e: e.matmul(sg[:, k * 512:k * 512 + N], lhsT=lh, rhs=rh_, start=True, stop=True), reads=[rk, rq], writes=[rs])

            def ex(g):
                sg, rs = SG[g % 2], r_SG[g % 2]
                ng = len(grp[g])
                P.op("act", lambda e: e.activation(out=PT3[g % 2][:, 0:ng, 0:N], in_=sg.rearrange("p (k n) -> p k n", k=3)[:, 0:ng, 0:N],
                                                   func=AF.Exp, scale=scale), reads=[rs], writes=[r_PT3[g % 2]])

            def pv(g):
                for k, (j, cx_) in enumerate(grp[g]):
                    ti = g * 3 + k
                    P.op("pe", lambda e: e.matmul(ps_o[0:65, 0:N], lhsT=vv[:, j, :], rhs=PT3[g % 2][:, k, 0:N], start=(ti == 0), stop=(ti == nt - 1)),
                         reads=[rv, r_PT3[g % 2]], writes=[r_o])
            qk(0)
            for g in range(G):
                if g + 1 < G:
                    qk(g + 1)
                ex(g)
                pv(g)

        def normalise(ps_o, r_o, N):
            u = ctr["u"] % 2
            ctr["u"] += 1
            P.op("dve", lambda e: e.tensor_copy(out=osb[u][:, 0:N], in_=ps_o[0:65, 0:N]), reads=[r_o], writes=[r_osb[u]])
            P.op("pe", lambda e: e.matmul(ps_o[0:64, 0:N], lhsT=sel[:, :], rhs=osb[u][:, 0:N], start=True, stop=True), reads=[r_osb[u], r_l], writes=[r_o])
            P.op("dve", lambda e: e.reciprocal(out=rec[u][:, 0:N], in_=ps_o[0:64, 0:N]), reads=[r_o], writes=[r_rec[u]])
            P.op("dve", lambda e: e.tensor_tensor(out=rec[u][:, 0:N], in0=rec[u][:, 0:N], in1=osb[u][0:64, 0:N], op=ALU.mult),
                 reads=[r_rec[u], r_osb[u]], writes=[r_rec[u]])
            return rec[u], r_rec[u]

        qgroups = [(t0, N, False) for (t0, N) in GROUPS[:8]] + ([] if last else [(T, CT, True)])
        o1all = sbuf(es, "o1all", [64, TT], F32)
        r_o1all = Res()
        units = [("da", h, m) for h in range(6) for m in range(2)] + [("ml", h, 0) for h in range(6)]
        for ui, (kind, h, m) in enumerate(units):
            bsel = ui % 2
            kt, rk, vv, rv, qt, rq = KT[bsel], r_KT[bsel], VV[bsel], r_VV[bsel], QT[bsel], r_QT[bsel]
            if kind == "da":
                r0 = (h * 2 + m) * 32
                for pg in range(3):
                    for rr in range(4):
                        dma("sp", kt[32 * pg:32 * pg + 32, rr * T:(rr + 1) * T], kda_all[((h * 2 + m) * 4 + rr) * 32:((h * 2 + m) * 4 + rr + 1) * 32, 0:T], reads=[rD["kda_all"]], writes=[rk])
                    dma("sp", kt[32 * pg:32 * pg + 32, S:S + CT], kda_own[r0:r0 + 32, T:TT], reads=[rD["kda_own"]], writes=[rk])
                    dma("sp", qt[32 * pg:32 * pg + 32, 0, :], qda[r0:r0 + 32, :], reads=[rD["qda"]], writes=[rq])
                    dma("sp", qt[32 * pg:32 * pg + 32, 1, :], qdaf[r0:r0 + 32, :], reads=[rD["qdaf"]], writes=[rq])
                va, vo, nva, nvo = vda_all, vda_own, "vda_all", "vda_own"
                dk, scale = 32, 32 ** -0.5
            else:
                for rr in range(4):
                    for half in range(2):
                        q0 = ((2 * h + half) * 4 + rr) * 48
                        dma("sp", kt[half * 48:(half + 1) * 48, rr * T:(rr + 1) * T], kml_all[q0:q0 + 48, 0:T], reads=[rD["kml_all"]], writes=[rk])
                dma("sp", kt[0:96, S:S + CT], kml_own[h * 96:(h + 1) * 96, T:TT], reads=[rD["kml_own"]], writes=[rk])
                dma("sp", qt[0:96, 0, :], qml[h], reads=[rD["qml"]], writes=[rq])
                dma("sp", qt[0:96, 1, :], qmlf[h], reads=[rD["qmlf"]], writes=[rq])
                va, vo, nva, nvo = vml_all, vml_own, "vml_all", "vml_own"
                dk, scale = 96, 96 ** -0.5
            for rr in range(4):
                row0 = (h * 4 + rr) * TT
                dma("sp", vv[rr * 32:(rr + 1) * 32, 0:128, :], va[row0:row0 + T, :].rearrange("(p j) e -> p j e", j=128), reads=[rD[nva]], writes=[rv])
            dma("sp", vv[:, 128:130, :], vo[h * TT + T:(h + 1) * TT, :].rearrange("(j p) e -> p j e", p=128), reads=[rD[nvo]], writes=[rv])
            for gi, (t0, N, isctx) in enumerate(qgroups):
                ps_o, r_o = psb[6 + gi % 2], psr[6 + gi % 2]
                attend(kt, rk, vv, rv, qt, rq, dk, scale, t0, N, isctx, ps_o, r_o)
                o1, ro1 = normalise(ps_o, r_o, N)
                if kind == "da" and m == 0:
                    P.op("pool", lambda e: e.tensor_copy(out=o1all[:, t0:t0 + N], in_=o1[:, 0:N]), reads=[ro1], writes=[r_o1all])
                    continue
                yi = ctr["y"] % 2
                ctr["y"] += 1
                if kind == "da":
                    P.op("dve", lambda e: e.scalar_tensor_tensor(out=od[:, 0:N], in0=o1[:, 0:N], scalar=lsc[0:64, 3:4], in1=o1all[:, t0:t0 + N],
                                                                 op0=ALU.mult, op1=ALU.add), reads=[ro1, r_o1all, r_l], writes=[r_od])
                    P.op("pool", lambda e: e.tensor_tensor(out=od2[:, 0:N], in0=od[:, 0:N], in1=od[:, 0:N], op=ALU.mult), reads=[r_od], writes=[r_od])
                    ps, pr = ps_o, r_o
                    P.op("pe", lambda e: e.matmul(ps[0:64, 0:N], lhsT=ones[0:64, 0:64], rhs=od2[:, 0:N], start=True, stop=True),
                         reads=[r_od, r_const], writes=[pr])
                    P.op("act", lambda e: e.activation(out=od2[:, 0:N], in_=ps[0:64, 0:N], func=AF.Sqrt, scale=1.0 / 64, bias=epsr[0:64, 0:1]),
                         reads=[pr, r_const], writes=[r_od])
                    P.op("dve", lambda e: e.reciprocal(out=od2[:, 0:N], in_=od2[:, 0:N]), reads=[r_od], writes=[r_od])
                    P.op("dve", lambda e: e.scalar_tensor_tensor(out=yb[yi][:, 0:N], in0=od[:, 0:N], scalar=gsub[:, 0:1], in1=od2[:, 0:N],
                                                                 op0=ALU.mult, op1=ALU.mult), reads=[r_od, r_l], writes=[r_yb[yi]])
                    slot = h
                else:
                    P.op("dve", lambda e: e.tensor_copy(out=yb[yi][:, 0:N], in_=o1[:, 0:N]), reads=[ro1], writes=[r_yb[yi]])
                    slot = 6 + h
                dma("pool", yT[slot, :, t0:t0 + N], yb[yi][:, 0:N], reads=[r_yb[yi]], writes=[rY])
        P.barrier()
        P.flush()
        es.close()

    def stage_C(l, last):
        es = ExitStack()
        modT, r_mod = compute_mod(es, l)
        xn, cxn = xnames(l)
        xon, cxon = xnames(l + 1)
        yT, xT, cxT = dget("yT"), dget(xn), dget(cxn)
        xTn = dget(xon)
        cxTn = None if last else dget(cxon)
        x1T = cx.dram("x1T_%d" % l, [8, 128, TT], F32)
        h2T = cx.dram("h2T_%d" % l, [8, 128, TT], BF16)
        gTd = cx.dram("gT_%d" % l, [16, TT], F32)
        rD = cx.res
        lnv = sbuf(es, "lnv", [128, 4, 8], F32)
        rw = sbuf(es, "rw", [128, 8, 16], F32)
        rb = sbuf(es, "rb", [128, 16], F32)
        selE = sbuf(es, "selE", [16, 16, 128], F32)
        r_c = Res()
        for i, nm in enumerate(("ln1_gT", "ln1_bT", "ln2_gT", "ln2_bT")):
            dma("sp", lnv[:, i, :], W[nm][l], writes=[r_c])
        dma("sp", rw[:], W["router_w"].rearrange("(c p) n -> p c n", p=128), writes=[r_c])
        dma("sp", rb[:], W["router_b"].partition_broadcast(128), writes=[r_c])
        for e_ in range(16):
            P.op("dve", lambda e, e_=e_: e.tensor_scalar_mul(out=selE[:, e_, :], in0=ones[0:16, :], scalar1=ident[0:16, e_:e_ + 1]),
                 reads=[r_const], writes=[r_c])
        groups = GROUPS[:8] + ([] if last else [GROUPS[8]])

        def layer_norm(u, r_u, N, gi_, bi_, outt, r_out, tmpA, tmpB, r_t):
            sqv = tmpA
            for c in range(8):
                P.op("pool", lambda e, c=c: e.tensor_tensor(out=sqv[:, c, 0:N], in0=u[:, c, 0:N], in1=u[:, c, 0:N], op=ALU.mult),
                     reads=[r_u], writes=[r_t])
            p1, r1 = next_ps()
            for c in range(8):
                P.op("pe", lambda e, c=c: e.matmul(p1[:, 0:N], lhsT=ones[:, :], rhs=u[:, c, 0:N], start=(c == 0), stop=(c == 7)),
                     reads=[r_u, r_const], writes=[r1])
            p2, r2 = next_ps()
            for c in range(8):
                P.op("pe", lambda e, c=c: e.matmul(p2[:, 0:N], lhsT=ones[:, :], rhs=sqv[:, c, 0:N], start=(c == 0), stop=(c == 7)),
                     reads=[r_t, r_const], writes=[r2])
            mean, msq, rs = tmpB[:, 0, :], tmpB[:, 1, :], tmpB[:, 2, :]
            r_b = Res()
            P.op("dve", lambda e: e.tensor_scalar_mul(out=mean[:, 0:N], in0=p1[:, 0:N], scalar1=1.0 / D), reads=[r1], writes=[r_b])
            P.op("pool", lambda e: e.tensor_tensor(out=msq[:, 0:N], in0=mean[:, 0:N], in1=mean[:, 0:N], op=ALU.mult), reads=[r_b], writes=[r_b])
            P.op("dve", lambda e: e.scalar_tensor_tensor(out=rs[:, 0:N], in0=p2[:, 0:N], scalar=1.0 / D, in1=msq[:, 0:N],
                                                         op0=ALU.mult, op1=ALU.subtract), reads=[r2, r_b], writes=[r_b])
            P.op("act", lambda e: e.activation(out=rs[:, 0:N], in_=rs[:, 0:N], func=AF.Sqrt, bias=epsr[:, 1:2], scale=1.0),
                 reads=[r_b, r_const], writes=[r_b])
            P.op("dve", lambda e: e.reciprocal(out=rs[:, 0:N], in_=rs[:, 0:N]), reads=[r_b], writes=[r_b])
            for c in range(8):
                P.op("pool", lambda e, c=c: e.tensor_tensor(out=sqv[:, c, 0:N], in0=u[:, c, 0:N], in1=mean[:, 0:N], op=ALU.subtract),
                     reads=[r_u, r_b, r_t], writes=[r_t])
                P.op("dve", lambda e, c=c: e.tensor_tensor(out=sqv[:, c, 0:N], in0=sqv[:, c, 0:N], in1=rs[:, 0:N], op=ALU.mult),
                     reads=[r_t, r_b], writes=[r_t])
                P.op("dve", lambda e, c=c: e.tensor_scalar(out=outt[:, c, 0:N], in0=sqv[:, c, 0:N], scalar1=lnv[:, gi_, c:c + 1],
                                                          scalar2=lnv[:, bi_, c:c + 1], op0=ALU.mult, op1=ALU.add),
                     reads=[r_t, r_c], writes=[r_out])

        with ExitStack() as e1:
            wout = sbuf(e1, "wout", [64, 16, D], BF16)
            stgo = [sbuf(e1, "stgo%d" % i, [64, D], F32) for i in range(2)]
            r_wo, r_so = Res(), [Res(), Res()]
            wov = W["w_out"][l].rearrange("(kt p) n -> p kt n", p=64)
            for kt in range(16):
                dma("sp", stgo[kt % 2][:], wov[:, kt, :], writes=[r_so[kt % 2]])
                P.op("dve", lambda e, kt=kt: e.tensor_copy(out=wout[:, kt, :], in_=stgo[kt % 2][:]), reads=[r_so[kt % 2]], writes=[r_wo])
            yg = [sbuf(e1, "yg%d" % i, [64, 16, 512], BF16) for i in range(2)]
            xg = [sbuf(e1, "xgc%d" % i, [128, 8, 512], F32) for i in range(2)]
            r_yg, r_xg = [Res(), Res()], [Res(), Res()]
            u = sbuf(e1, "u", [128, 8, 512], F32)
            tA = sbuf(e1, "tA", [128, 8, 512], F32)
            tB = sbuf(e1, "tB", [128, 3, 512], F32)
            x1 = sbuf(e1, "x1", [128, 8, 512], F32)
            h2f = sbuf(e1, "h2f", [128, 8, 512], F32)
            h2b = sbuf(e1, "h2b", [128, 8, 512], BF16)
            gts = sbuf(e1, "gts", [16, 512], F32)
            rt = sbuf(e1, "rt", [128, 160], F32)
            r_u, r_tA, r_x1, r_h2f, r_h2b, r_gts, r_rt = Res(), Res(), Res(), Res(), Res(), Res(), Res()
            for gi, (t0, N) in enumerate(groups):
                b = gi % 2
                isctx = (t0 == T)
                mi = 1 if isctx else 0
                dma("sp", yg[b][:, :, 0:N], yT.rearrange("s p t -> p s t")[:, :, t0:t0 + N], reads=[rD["yT"]], writes=[r_yg[b]])
                src = cxT.rearrange("c p t -> p c t") if isctx else xT.rearrange("c p t -> p c t")[:, :, t0:t0 + N]
                dma("sp", xg[b][:, :, 0:N], src, reads=[rD[cxn if isctx else xn]], writes=[r_xg[b]])
                P.op("pool", lambda e: e.tensor_scalar_mul(out=xg[b][:, :, 0:N], in0=xg[b][:, :, 0:N], scalar1=float(ALPHA)),
                     reads=[r_xg[b]], writes=[r_xg[b]])
                for oc in range(8):
                    ps, pr = next_ps()
                    for kt in range(16):
                        P.op("pe", lambda e, kt=kt: e.matmul(ps[:, 0:N], lhsT=wout[:, kt, oc * 128:(oc + 1) * 128], rhs=yg[b][:, kt, 0:N],
                                                            start=(kt == 0), stop=(kt == 15)), reads=[r_wo, r_yg[b]], writes=[pr])
                    P.op("dve", lambda e: e.scalar_tensor_tensor(out=u[:, oc, 0:N], in0=ps[:, 0:N], scalar=modT[:, 16 + oc, mi:mi + 1],
                                                                 in1=xg[b][:, oc, 0:N], op0=ALU.mult, op1=ALU.add),
                         reads=[pr, r_mod, r_xg[b]], writes=[r_u])
                layer_norm(u, r_u, N, 0, 1, x1, r_x1, tA, tB, r_tA)
                dma("act", x1T.rearrange("c p t -> p c t")[:, :, t0:t0 + N], x1[:, :, 0:N], reads=[r_x1], writes=[rD["x1T_%d" % l]])
                for c in range(8):
                    P.op("dve", lambda e, c=c: e.tensor_scalar(out=h2f[:, c, 0:N], in0=x1[:, c, 0:N], scalar1=modT[:, 32 + c, mi:mi + 1],
                                                              scalar2=modT[:, 24 + c, mi:mi + 1], op0=ALU.mult, op1=ALU.add),
                         reads=[r_x1, r_mod], writes=[r_h2f])
                    P.op("pool", lambda e, c=c: e.tensor_copy(out=h2b[:, c, 0:N], in_=h2f[:, c, 0:N]), reads=[r_h2f], writes=[r_h2b])
                dma("act", h2T.rearrange("c p t -> p c t")[:, :, t0:t0 + N], h2b[:, :, 0:N], reads=[r_h2b], writes=[rD["h2T_%d" % l]])
                for j in range(N // 128):
                    ts = slice(j * 128, (j + 1) * 128)
                    ps, pr = next_ps()
                    for c in range(8):
                        P.op("pe", lambda e, c=c: e.matmul(ps[:, 0:16], lhsT=h2f[:, c, ts], rhs=rw[:, c, :], start=(c == 0), stop=(c == 7)),
                             reads=[r_h2f, r_c], writes=[pr])
                    sc_, ch, mc, sel1, tmpv = rt[:, 0:16], rt[:, 16:32], rt[:, 32:48], rt[:, 48:64], rt[:, 64:80]
                    q4 = rt[:, 80:112].rearrange("p (a b) -> p a b", a=8)
                    s1 = rt[:, 112:120]
                    ch4 = ch.rearrange("p (g i) -> p g i", g=4)
                    mc4 = mc.rearrange("p (g i) -> p g i", g=4)
                    R_ = [r_rt]

                    def dv(fn):
                        P.op("dve", fn, reads=R_ + [r_c], writes=R_)
                    P.op("act", lambda e: e.activation(out=sc_, in_=ps[:, 0:16], func=AF.Sigmoid), reads=[pr], writes=R_)
                    dv(lambda e: e.tensor_tensor(out=ch, in0=sc_, in1=rb[:, :], op=ALU.add))
                    dv(lambda e: e.tensor_tensor(out=q4[:, 0, :], in0=ch4[:, :, 0], in1=ch4[:, :, 1], op=ALU.max))
                    dv(lambda e: e.tensor_tensor(out=q4[:, 1, :], in0=ch4[:, :, 0], in1=ch4[:, :, 1], op=ALU.min))
                    dv(lambda e: e.tensor_tensor(out=q4[:, 2, :], in0=ch4[:, :, 2], in1=ch4[:, :, 3], op=ALU.max))
                    dv(lambda e: e.tensor_tensor(out=q4[:, 3, :], in0=ch4[:, :, 2], in1=ch4[:, :, 3], op=ALU.min))
                    dv(lambda e: e.tensor_tensor(out=q4[:, 4, :], in0=q4[:, 0, :], in1=q4[:, 2, :], op=ALU.max))
                    dv(lambda e: e.tensor_tensor(out=q4[:, 5, :], in0=q4[:, 0, :], in1=q4[:, 2, :], op=ALU.min))
                    dv(lambda e: e.tensor_tensor(out=q4[:, 6, :], in0=q4[:, 1, :], in1=q4[:, 3, :], op=ALU.max))
                    dv(lambda e: e.tensor_tensor(out=q4[:, 5, :], in0=q4[:, 5, :], in1=q4[:, 6, :], op=ALU.max))
                    dv(lambda e: e.tensor_tensor(out=q4[:, 4, :], in0=q4[:, 4, :], in1=q4[:, 5, :], op=ALU.add))
                    dv(lambda e: e.reduce_max(out=s1[:, 0:1], in_=q4[:, 4, :], axis=AX.X))
                    dv(lambda e: e.tensor_scalar(out=q4[:, 7, :], in0=q4[:, 4, :], scalar1=s1[:, 0:1], scalar2=None, op0=ALU.is_equal))
                    dv(lambda e: e.tensor_scalar(out=q4[:, 7, :], in0=q4[:, 7, :], scalar1=-1.0, scalar2=1.0e9, op0=ALU.add, op1=ALU.mult))
                    for i in range(4):
                        dv(lambda e, i=i: e.tensor_tensor(out=mc4[:, :, i], in0=ch4[:, :, i], in1=q4[:, 7, :], op=ALU.add))
                    dv(lambda e: e.reduce_max(out=s1[:, 1:2], in_=mc, axis=AX.X))
                    dv(lambda e: e.tensor_scalar(out=sel1, in0=mc, scalar1=s1[:, 1:2], scalar2=None, op0=ALU.is_equal))
                    dv(lambda e: e.scalar_tensor_tensor(out=mc, in0=sel1, scalar=-1.0e9, in1=mc, op0=ALU.mult, op1=ALU.add))
                    dv(lambda e: e.reduce_max(out=s1[:, 2:3], in_=mc, axis=AX.X))
                    dv(lambda e: e.tensor_scalar(out=tmpv, in0=mc, scalar1=s1[:, 2:3], scalar2=None, op0=ALU.is_equal))
                    dv(lambda e: e.tensor_tensor(out=sel1, in0=sel1, in1=tmpv, op=ALU.add))
                    dv(lambda e: e.tensor_tensor(out=sel1, in0=sel1, in1=sc_, op=ALU.mult))
                    dv(lambda e: e.reduce_sum(out=s1[:, 3:4], in_=sel1, axis=AX.X))
                    dv(lambda e: e.reciprocal(out=s1[:, 3:4], in_=s1[:, 3:4]))
                    dv(lambda e: e.tensor_scalar_mul(out=sel1, in0=sel1, scalar1=s1[:, 3:4]))
                    ps2, pr2 = next_ps()
                    P.op("pe", lambda e: e.transpose(ps2[0:16, 0:128], sel1, ident[:, :]), reads=R_ + [r_const], writes=[pr2])
                    P.op("dve", lambda e: e.tensor_copy(out=gts[:, ts], in_=ps2[0:16, 0:128]), reads=[pr2], writes=[r_gts])
                dma("act", gTd[:, t0:t0 + N], gts[:, 0:N], reads=[r_gts], writes=[rD["gT_%d" % l]])
            P.barrier()

        blocks = [(0, 1024), (1024, 1024), (2048, 1024), (3072, 1024 if last else 1024 + CT)]
        NBM = 1024 + CT
        with ExitStack() as e2:
            h2k = sbuf(e2, "h2k", [128, 8, NBM], BF16)
            gk = sbuf(e2, "gk", [16, NBM], F32)
            acc = sbuf(e2, "acc", [128, 8, NBM], F32)
            r_h2k, r_gk, r_acc = Res(), Res(), Res()
            for (b0, NB) in blocks:
                subs = [(o, min(512, NB - o)) for o in range(0, NB, 512)]
                dma("sp", h2k[:, :, 0:NB], h2T.rearrange("c p t -> p c t")[:, :, b0:b0 + NB], reads=[rD["h2T_%d" % l]], writes=[r_h2k])
                dma("sp", gk[:, 0:NB], gTd[:, b0:b0 + NB], reads=[rD["gT_%d" % l]], writes=[r_gk])
                with ExitStack() as eE:
                    w1b = [sbuf(eE, "w1b%d" % i, [128, 8, 512], BF16) for i in range(2)]
                    w3b = [sbuf(eE, "w3b%d" % i, [128, 8, 512], BF16) for i in range(2)]
                    w2b = [sbuf(eE, "w2b%d" % i, [128, 4, D], BF16) for i in range(2)]
                    r_w1, r_w3, r_w2 = [Res(), Res()], [Res(), Res()], [Res(), Res()]
                    gbc = [sbuf(eE, "gbc%d" % i, [128, 512], F32) for i in range(2)]
                    sa = [sbuf(eE, "sa%d" % i, [128, 512], F32) for i in range(4)]
                    gb = [sbuf(eE, "gb%d" % i, [128, 512], F32) for i in range(4)]
                    gT_ = [sbuf(eE, "gT_%d" % i, [128, 4, 512], BF16) for i in range(2)]
                    r_gbc, r_gT = [Res(), Res()], [Res(), Res()]
                    r_sa, r_gb = [Res() for _ in range(4)], [Res() for _ in range(4)]
                    cnt4 = {"i": 0}

                    def wload(e_):
                        k = e_ % 2
                        dma("pool", w1b[k][:], W["exp_w1"][l, e_].rearrange("(c p) n -> p c n", p=128), writes=[r_w1[k]])
                        dma("pool", w3b[k][:], W["exp_w3"][l, e_].rearrange("(c p) n -> p c n", p=128), writes=[r_w3[k]])
                        dma("pool", w2b[k][:], W["exp_w2"][l, e_].rearrange("(c p) n -> p c n", p=128), writes=[r_w2[k]])

                    items = [(e_, o, n) for e_ in range(NE) for (o, n) in subs]

                    def s1(i):
                        e_, o, n = items[i]
                        k = i % 2
                        kw_ = e_ % 2
                        ps, pr = next_ps(0, 8)
                        P.op("pe", lambda e: e.matmul(ps[:, 0:n], lhsT=selE[:, e_, :], rhs=gk[:, o:o + n], start=True, stop=True),
                             reads=[r_gk, r_c], writes=[pr])
                        P.op("act", lambda e: e.copy(out=gbc[k][:, 0:n], in_=ps[:, 0:n]), reads=[pr], writes=[r_gbc[k]])
                        for fc in range(4):
                            q_ = cnt4["i"] % 4
                            cnt4["i"] += 1
                            pa, ra = next_ps(0, 8)
                            for c in range(8):
                                P.op("pe", lambda e, c=c: e.matmul(pa[:, 0:n], lhsT=w1b[kw_][:, c, fc * 128:(fc + 1) * 128], rhs=h2k[:, c, o:o + n],
                                                                  start=(c == 0), stop=(c == 7)), reads=[r_w1[kw_], r_h2k], writes=[ra])
                            pb, rb_ = next_ps(0, 8)
                            for c in range(8):
                                P.op("pe", lambda e, c=c: e.matmul(pb[:, 0:n], lhsT=w3b[kw_][:, c, fc * 128:(fc + 1) * 128], rhs=h2k[:, c, o:o + n],
                                                                  start=(c == 0), stop=(c == 7)), reads=[r_w3[kw_], r_h2k], writes=[rb_])
                            P.op("act", lambda e: e.activation(out=sa[q_][:, 0:n], in_=pa[:, 0:n], func=AF.Silu), reads=[ra], writes=[r_sa[q_]])
                            P.op("dve", lambda e: e.tensor_tensor(out=gb[q_][:, 0:n], in0=pb[:, 0:n], in1=gbc[k][:, 0:n], op=ALU.mult),
                                 reads=[rb_, r_gbc[k]], writes=[r_gb[q_]])
                            P.op("pool", lambda e: e.tensor_tensor(out=gT_[k][:, fc, 0:n], in0=sa[q_][:, 0:n], in1=gb[q_][:, 0:n], op=ALU.mult),
                                 reads=[r_sa[q_], r_gb[q_]], writes=[r_gT[k]])

                    def s2(i):
                        e_, o, n = items[i]
                        k = i % 2
                        kw_ = e_ % 2
                        for oc in range(8):
                            pf, rf = next_ps(0, 8)
                            for fc in range(4):
                                P.op("pe", lambda e, fc=fc: e.matmul(pf[:, 0:n], lhsT=w2b[kw_][:, fc, oc * 128:(oc + 1) * 128], rhs=gT_[k][:, fc, 0:n],
                                                                    start=(fc == 0), stop=(fc == 3)), reads=[r_w2[kw_], r_gT[k]], writes=[rf])
                            if e_ == 0:
                                P.op("dve", lambda e: e.tensor_copy(out=acc[:, oc, o:o + n], in_=pf[:, 0:n]), reads=[rf], writes=[r_acc])
                            else:
                                P.op("dve", lambda e: e.tensor_tensor(out=acc[:, oc, o:o + n], in0=acc[:, oc, o:o + n], in1=pf[:, 0:n], op=ALU.add),
                                     reads=[rf, r_acc], writes=[r_acc])
                    wload(0)
                    wload(1)
                    s1(0)
                    for i in range(len(items)):
                        if i + 1 < len(items):
                            s1(i + 1)
                        s2(i)
                        e_cur = items[i][0]
                        if (i + 1 == len(items) or items[i + 1][0] != e_cur) and e_cur + 2 < NE:
                            wload(e_cur + 2)
                    P.barrier()
                with ExitStack() as e3:
                    x1g = sbuf(e3, "x1g", [128, 8, 512], F32)
                    tA2 = sbuf(e3, "tA2", [128, 8, 512], F32)
                    tB2 = sbuf(e3, "tB2", [128, 3, 512], F32)
                    xo = sbuf(e3, "xo", [128, 8, 512], F32)
                    r_x1g, r_tA2, r_xo = Res(), Res(), Res()
                    u2, r_u2 = x1g, r_x1g
                    for (o, n) in subs:
                        t0 = b0 + o
                        isctx = (t0 >= T)
                        mi = 1 if isctx else 0
                        dma("sp", x1g[:, :, 0:n], x1T.rearrange("c p t -> p c t")[:, :, t0:t0 + n], reads=[rD["x1T_%d" % l]], writes=[r_x1g])
                        P.op("pool", lambda e: e.tensor_scalar_mul(out=x1g[:, :, 0:n], in0=x1g[:, :, 0:n], scalar1=float(ALPHA)),
                             reads=[r_x1g], writes=[r_x1g])
                        for c in range(8):
                            P.op("dve", lambda e, c=c: e.scalar_tensor_tensor(out=u2[:, c, 0:n], in0=acc[:, c, o:o + n], scalar=modT[:, 40 + c, mi:mi + 1],
                                                                             in1=x1g[:, c, 0:n], op0=ALU.mult, op1=ALU.add),
                                 reads=[r_acc, r_mod, r_x1g], writes=[r_u2])
                        layer_norm(u2, r_u2, n, 2, 3, xo, r_xo, tA2, tB2, r_tA2)
                        if isctx:
                            dma("act", cxTn.rearrange("c p t -> p c t"), xo[:, :, 0:n], reads=[r_xo], writes=[rD[cxon]])
                        else:
                            dma("act", xTn.rearrange("c p t -> p c t")[:, :, t0:t0 + n], xo[:, :, 0:n], reads=[r_xo], writes=[rD[xon]])
                    P.barrier()
        P.flush()
        es.close()

    def stage_R(l, last):
        es = ExitStack()
        lg, r_lg = load_lg(es, l)
        yT = dget("yT")
        rD = cx.res
        qr, kr, krt, vr, sg, rsum_all = dget("qr"), dget("kr"), dget("krt"), dget("vr"), dget("sg"), dget("rsum_all")
        NCH = TT // 128
        cst = sbuf(es, "cst", [128, 4, 128], F32)
        tpos = sbuf(es, "tposr", [128, 32], F32)
        eb = sbuf(es, "eb", [128, 16], F32)
        r_k = Res()
        dma("sp", cst[:], W["rtabs"].rearrange("a p c -> p a c"), writes=[r_k])
        dma("sp", tpos[:], W["tpos"][:, :], writes=[r_k])
        dma("sp", eb[:], W["ebound"][:, :], writes=[r_k])
        MT = sbuf(es, "MT", [128, 128], F32)
        xi = sbuf(es, "xi", [64, 2, 128], F32)
        zeta = sbuf(es, "zeta", [128, 4], F32)
        zc = sbuf(es, "zc", [128, 4], F32)
        cdec = sbuf(es, "cdec", [64, 2], F32)
        coef = sbuf(es, "coef", [64, 16], F32)
        tmpm = sbuf(es, "tmpm", [128, 128], F32)
        p127 = sbuf(es, "p127", [128, 4], F32)
        qT = sbuf(es, "qTr", [64, TT], BF16)
        kT = sbuf(es, "kTr", [64, TT], BF16)
        ktm = sbuf(es, "ktm", [128, NCH, 64], BF16)
        vtm = sbuf(es, "vtm", [128, NCH, 64], BF16)
        kz = sbuf(es, "kz", [128, NCH, 2, 64], BF16)
        sgT = sbuf(es, "sgT", [64, TT], BF16)
        rs_all = sbuf(es, "rs_all", [64, 4, 512], F32)
        Pst = sbuf(es, "Pst", [64, NCH + 1, 64], F32)
        Nst = sbuf(es, "Nst", [64, NCH + 1, 64], F32)
        Pb = sbuf(es, "Pb", [64, NCH + 1, 64], BF16)
        Nb = sbuf(es, "Nb", [64, NCH + 1, 64], BF16)
        am = [sbuf(es, "am%d" % i, [128, 128], BF16) for i in range(2)]
        qx = [sbuf(es, "qx%d" % i, [64, 2, 128], BF16) for i in range(2)]
        osb_ = sbuf(es, "osbr", [64, 512], F32)
        osq = sbuf(es, "osq", [64, 512], F32)
        yb = sbuf(es, "ybr", [64, 512], BF16)
        r_q, r_kk, r_ktm, r_vtm, r_kz, r_sg, r_rs, r_st, r_o, r_yb = (Res() for _ in range(10))
        r_am, r_qx = [Res(), Res()], [Res(), Res()]
        dma("sp", rs_all[:], rsum_all.rearrange("(r p) c -> p r c", p=64), reads=[rD["rsum_all"]], writes=[r_rs])
        P.op("dve", lambda e: e.tensor_scalar(out=p127[:, 0:1], in0=tpos[:, 0:1], scalar1=-1.0, scalar2=127.0, op0=ALU.mult, op1=ALU.add), reads=[r_k], writes=[r_k])
        P.op("dve", lambda e: e.tensor_scalar(out=p127[:, 2:3], in0=tpos[:, 0:1], scalar1=-1.0, scalar2=255.0, op0=ALU.mult, op1=ALU.add), reads=[r_k], writes=[r_k])
        for h in range(4):
            lf, lb = lg[:, h:h + 1], lg[:, 4 + h:5 + h]
            RK = [r_k, r_lg]
            P.op("dve", lambda e: e.tensor_scalar_mul(out=tmpm[:], in0=cst[:, 0, :], scalar1=lf), reads=RK, writes=[r_k])
            P.op("dve", lambda e: e.scalar_tensor_tensor(out=tmpm[:], in0=cst[:, 1, :], scalar=lb, in1=tmpm[:], op0=ALU.mult, op1=ALU.add), reads=RK, writes=[r_k])
            P.op("act", lambda e: e.activation(out=MT[:], in_=tmpm[:], func=AF.Exp), reads=RK, writes=[r_k])
            P.op("act", lambda e: e.activation(out=xi[:, 0, :], in_=cst[0:64, 2, :], func=AF.Exp, scale=lg[0:64, h:h + 1]), reads=RK, writes=[r_k])
            P.op("act", lambda e: e.activation(out=xi[:, 1, :], in_=cst[0:64, 3, :], func=AF.Exp, scale=lg[0:64, 4 + h:5 + h]), reads=RK, writes=[r_k])
            P.op("act", lambda e: e.activation(out=zeta[:, 0:1], in_=p127[:, 0:1], func=AF.Exp, scale=lf), reads=RK, writes=[r_k])
            P.op("act", lambda e: e.activation(out=zeta[:, 1:2], in_=tpos[:, 0:1], func=AF.Exp, scale=lb), reads=RK, writes=[r_k])
            P.op("act", lambda e: e.activation(out=zc[:, 0:1], in_=p127[:, 2:3], func=AF.Exp, scale=lf), reads=RK, writes=[r_k])
            P.op("act", lambda e: e.activation(out=zc[:, 1:2], in_=p127[:, 0:1], func=AF.Exp, scale=lf), reads=RK, writes=[r_k])
            P.op("act", lambda e: e.activation(out=zc[:, 2:4], in_=tpos[:, 0:2], func=AF.Exp, scale=lb), reads=RK, writes=[r_k])
            P.op("act", lambda e: e.activation(out=coef[:, 0:16], in_=eb[0:64, :], func=AF.Exp, scale=lg[0:64, h:h + 1]), reads=RK, writes=[r_k])
            P.op("act", lambda e: e.activation(out=coef[:, 4:8], in_=eb[0:64, 4:8], func=AF.Exp, scale=lg[0:64, 4 + h:5 + h]), reads=RK, writes=[r_k])
            P.op("act", lambda e: e.activation(out=coef[:, 9:10], in_=eb[0:64, 9:10], func=AF.Exp, scale=lg[0:64, 4 + h:5 + h]), reads=RK, writes=[r_k])
            P.op("act", lambda e: e.activation(out=cdec[:, 0:1], in_=cst[0:64, 3, 0:1], func=AF.Exp, scale=lg[0:64, h:h + 1]), reads=RK, writes=[r_k])
            P.op("act", lambda e: e.activation(out=cdec[:, 1:2], in_=cst[0:64, 3, 0:1], func=AF.Exp, scale=lg[0:64, 4 + h:5 + h]), reads=RK, writes=[r_k])
            dma("sp", qT[:], qr[h], reads=[rD["qr"]], writes=[r_q])
            dma("sp", kT[:], kr[h], reads=[rD["kr"]], writes=[r_kk])
            dma("sp", sgT[:], sg[h], reads=[rD["sg"]], writes=[r_sg])
            dma("sp", ktm[:], krt.rearrange("(j p) c -> p j c", p=128)[:, :, h * 64:(h + 1) * 64], reads=[rD["krt"]], writes=[r_ktm])
            dma("sp", vtm[:], vr.rearrange("(j p) c -> p j c", p=128)[:, :, h * 64:(h + 1) * 64], reads=[rD["vr"]], writes=[r_vtm])
            for d_ in range(2):
                P.op("pool", lambda e, d_=d_: e.tensor_scalar_mul(out=kz[:, :, d_, :], in0=ktm[:], scalar1=zeta[:, d_:d_ + 1]),
                     reads=[r_ktm, r_k], writes=[r_kz])
            kzc = sbuf(es, "kzc%d" % h, [128, 2, 2, 64], BF16)
            r_kzc = Res()
            for jt in range(2):
                P.op("pool", lambda e, jt=jt: e.tensor_scalar_mul(out=kzc[:, jt, 0, :], in0=ktm[:, 32 + jt, :], scalar1=zc[:, jt:jt + 1]), reads=[r_ktm, r_k], writes=[r_kzc])
                P.op("pool", lambda e, jt=jt: e.tensor_scalar_mul(out=kzc[:, jt, 1, :], in0=ktm[:, 32 + jt, :], scalar1=zc[:, 2 + jt:3 + jt]), reads=[r_ktm, r_k], writes=[r_kzc])
            pc, rc = next_ps()
            for d_ in range(2):
                for jt in range(2):
                    P.op("pe", lambda e, d_=d_, jt=jt: e.matmul(pc[0:64, d_ * 64:(d_ + 1) * 64], lhsT=kzc[:, jt, d_, :], rhs=vtm[:, 32 + jt, :],
                                                               start=(jt == 0), stop=(jt == 1)), reads=[r_kzc, r_vtm], writes=[rc])
            RS = [r_rs, r_k, r_st]
            P.op("dve", lambda e: e.tensor_scalar_mul(out=Pst[:, 0, :], in0=pc[0:64, 0:64], scalar1=coef[:, 8:9]), reads=[rc] + RS, writes=[r_st])
            P.op("dve", lambda e: e.tensor_scalar_mul(out=Nst[:, 32, :], in0=pc[0:64, 64:128], scalar1=coef[:, 9:10]), reads=[rc] + RS, writes=[r_st])
            for rp in range(4):
                P.op("dve", lambda e, rp=rp: e.scalar_tensor_tensor(out=Pst[:, 0, :], in0=rs_all[:, rp, h * 64:(h + 1) * 64], scalar=coef[:, rp:rp + 1],
                                                                   in1=Pst[:, 0, :], op0=ALU.mult, op1=ALU.add), reads=RS, writes=[r_st])
                P.op("dve", lambda e, rp=rp: e.scalar_tensor_tensor(out=Nst[:, 32, :], in0=rs_all[:, rp, (4 + h) * 64:(5 + h) * 64], scalar=coef[:, 4 + rp:5 + rp],
                                                                   in1=Nst[:, 32, :], op0=ALU.mult, op1=ALU.add), reads=RS, writes=[r_st])
            for n0 in range(0, 32, 4):
                pu, ru = next_ps()
                for n in range(n0, n0 + 4):
                    for d_ in range(2):
                        cc = ((n - n0) * 2 + d_) * 64
                        P.op("pe", lambda e, n=n, d_=d_, cc=cc: e.matmul(pu[0:64, cc:cc + 64], lhsT=kz[:, n, d_, :], rhs=vtm[:, n, :], start=True, stop=True),
                             reads=[r_kz, r_vtm], writes=[ru])
                P.op("dve", lambda e, n0=n0: e.tensor_copy(out=osq[:, 0:512], in_=pu[0:64, 0:512]), reads=[ru, r_o], writes=[r_o])
                for n in range(n0, n0 + 4):
                    cc = (n - n0) * 128
                    P.op("pool", lambda e, n=n, cc=cc: e.tensor_copy(out=Pst[:, n + 1, :], in_=osq[:, cc:cc + 64]), reads=[r_o, r_st], writes=[r_st])
                    P.op("pool", lambda e, n=n, cc=cc: e.tensor_copy(out=Nst[:, n, :], in_=osq[:, cc + 64:cc + 128]), reads=[r_o, r_st], writes=[r_st])
            for n in range(32):
                if n < 31:
                    P.op("dve", lambda e, n=n: e.scalar_tensor_tensor(out=Pst[:, n + 1, :], in0=Pst[:, n, :], scalar=cdec[:, 0:1], in1=Pst[:, n + 1, :],
                                                                     op0=ALU.mult, op1=ALU.add), reads=[r_st, r_k], writes=[r_st])
            for n in range(31, 0, -1):
                P.op("dve", lambda e, n=n: e.scalar_tensor_tensor(out=Nst[:, n, :], in0=Nst[:, n + 1, :], scalar=cdec[:, 1:2], in1=Nst[:, n, :],
                                                                 op0=ALU.mult, op1=ALU.add), reads=[r_st, r_k], writes=[r_st])
            pu, ru = next_ps()
            P.op("pe", lambda e: e.matmul(pu[0:64, 0:64], lhsT=kz[:, 32, 0, :], rhs=vtm[:, 32, :], start=True, stop=True), reads=[r_kz, r_vtm], writes=[ru])
            P.op("pe", lambda e: e.matmul(pu[0:64, 64:128], lhsT=kz[:, 33, 1, :], rhs=vtm[:, 33, :], start=True, stop=True), reads=[r_kz, r_vtm], writes=[ru])
            P.op("pool", lambda e: e.memset(Pb[:, 32, :], 0.0), reads=[r_st], writes=[r_st])
            P.op("pool", lambda e: e.memset(Nb[:, 34, :], 0.0), reads=[r_st], writes=[r_st])
            P.op("dve", lambda e: e.tensor_copy(out=Pb[:, 33, :], in_=pu[0:64, 0:64]), reads=[ru, r_st], writes=[r_st])
            P.op("dve", lambda e: e.tensor_copy(out=Nb[:, 33, :], in_=pu[0:64, 64:128]), reads=[ru, r_st], writes=[r_st])
            P.op("dve", lambda e: e.tensor_copy(out=Pb[:, 0:32, :], in_=Pst[:, 0:32, :]), reads=[r_st], writes=[r_st])
            P.op("dve", lambda e: e.tensor_copy(out=Nb[:, 1:33, :], in_=Nst[:, 1:33, :]), reads=[r_st], writes=[r_st])
            nchunks = 32 if last else 34
            for n0 in range(0, nchunks, 4):
                nn = min(4, nchunks - n0)
                N = nn * 128
                po, ro = next_ps()
                for n in range(n0, n0 + nn):
                    k = n % 2
                    tsl = slice(n * 128, (n + 1) * 128)
                    pa, ra = next_ps()
                    P.op("pe", lambda e: e.matmul(pa[:, 0:128], lhsT=kT[:, tsl], rhs=qT[:, tsl], start=True, stop=True), reads=[r_kk, r_q], writes=[ra])
                    P.op("dve", lambda e: e.tensor_tensor(out=am[k][:], in0=pa[:, 0:128], in1=MT[:], op=ALU.mult), reads=[ra, r_k], writes=[r_am[k]])
                    P.op("pool", lambda e: e.tensor_tensor(out=qx[k][:, 0, :], in0=qT[:, tsl], in1=xi[:, 0, :], op=ALU.mult), reads=[r_q, r_k], writes=[r_qx[k]])
                    P.op("pool", lambda e: e.tensor_tensor(out=qx[k][:, 1, :], in0=qT[:, tsl], in1=xi[:, 1, :], op=ALU.mult), reads=[r_q, r_k], writes=[r_qx[k]])
                    oc = (n - n0) * 128
                    P.op("pe", lambda e: e.matmul(po[0:64, oc:oc + 128], lhsT=vtm[:, n, :], rhs=am[k][:], start=True, stop=False), reads=[r_vtm, r_am[k]], writes=[ro])
                    P.op("pe", lambda e: e.matmul(po[0:64, oc:oc + 128], lhsT=Pb[:, n, :], rhs=qx[k][:, 0, :], start=False, stop=False), reads=[r_st, r_qx[k]], writes=[ro])
                    P.op("pe", lambda e: e.matmul(po[0:64, oc:oc + 128], lhsT=Nb[:, n + 1, :], rhs=qx[k][:, 1, :], start=False, stop=True), reads=[r_st, r_qx[k]], writes=[ro])
                t0 = n0 * 128
                P.op("dve", lambda e: e.tensor_copy(out=osb_[:, 0:N], in_=po[0:64, 0:N]), reads=[ro, r_o], writes=[r_o])
                P.op("pool", lambda e: e.tensor_tensor(out=osq[:, 0:N], in0=osb_[:, 0:N], in1=osb_[:, 0:N], op=ALU.mult), reads=[r_o], writes=[r_o])
                pq, rq_ = next_ps()
                P.op("pe", lambda e: e.matmul(pq[0:64, 0:N], lhsT=ones[0:64, 0:64], rhs=osq[:, 0:N], start=True, stop=True), reads=[r_o, r_const], writes=[rq_])
                P.op("act", lambda e: e.activation(out=osq[:, 0:N], in_=pq[0:64, 0:N], func=AF.Sqrt, scale=1.0 / 64, bias=epsr[0:64, 0:1]), reads=[rq_, r_const, r_o], writes=[r_o])
                P.op("dve", lambda e: e.reciprocal(out=osq[:, 0:N], in_=osq[:, 0:N]), reads=[r_o], writes=[r_o])
                P.op("dve", lambda e: e.tensor_tensor(out=osb_[:, 0:N], in0=osb_[:, 0:N], in1=osq[:, 0:N], op=ALU.mult), reads=[r_o], writes=[r_o])
                P.op("dve", lambda e: e.tensor_tensor(out=yb[:, 0:N], in0=osb_[:, 0:N], in1=sgT[:, t0:t0 + N], op=ALU.mult), reads=[r_o, r_sg], writes=[r_yb])
                dma("sp", yT[12 + h, :, t0:t0 + N], yb[:, 0:N], reads=[r_yb], writes=[rD["yT"]])
        P.barrier()
        P.flush()
        es.close()

    for (st, l) in stages:
        if st == "A":
            stage_A(l)
        elif st == "X":
            stage_X(l)
        elif st == "B":
            stage_B(l, l == DEPTH - 1)
        elif st == "R":
            stage_R(l, l == DEPTH - 1)
        else:
            stage_C(l, l == DEPTH - 1)
    fin = list(cx.out_tickets)
    print("program ops:", P.nops, "sems:", len(P.semkeys), flush=True)
    P.barrier()
    t = P.op("sp", lambda e: e.dma_start(out=ident[:], in_=W["ident"][:, :]), writes=[r_const])
    P.flush([t] + fin)
    P.es.close()
    es_glob.close()
    return nc


def rope_tables(r):
    pos = np.arange(r * T, (r + 1) * T)
    row = (pos // 64).astype(np.float32)
    col = (pos % 64).astype(np.float32)

    def tab(rot_dim):
        nf = rot_dim // 4
        freqs = (ROPE_BASE ** (-np.arange(nf, dtype=np.float32) / nf)).astype(np.float32)
        ang = np.concatenate([row[:, None] * freqs, col[:, None] * freqs], -1)
        c = np.repeat(np.cos(ang), 2, axis=1).T
        s_ = np.repeat(np.sin(ang), 2, axis=1).T
        return c.astype(np.float32), s_.astype(np.float32)
    cA, sA = tab(32)
    cR, sR = tab(64)

    def withctx(c, s_):
        c = np.concatenate([c, np.ones((c.shape[0], CT), np.float32)], 1)
        s_ = np.concatenate([s_, np.zeros((s_.shape[0], CT), np.float32)], 1)
        return np.stack([c, s_], 0)
    ropeA = withctx(np.tile(cA, (4, 1)), np.tile(sA, (4, 1)))
    cM = np.concatenate([np.ones((64, T), np.float32), cA], 0)
    sM = np.concatenate([np.zeros((64, T), np.float32), sA], 0)
    ropeM = withctx(cM, sM)
    ropeR = withctx(cR, sR)
    return ropeA, ropeM, ropeR


def host_weights(inp, core):
    b, r = core // 4, core % 4
    f = np.float32
    w = {}
    cv = np.stack([inp["c"][b].reshape(8, 128).T, inp["c_ctx"].reshape(8, 128).T], -1)
    w["cvec"] = np.ascontiguousarray(cv, f)
    w["w_ada"] = inp["w_ada"]
    w["b_adaT"] = np.ascontiguousarray(inp["b_ada"].reshape(DEPTH, 48, 128).transpose(0, 2, 1))
    w["w_in"] = inp["w_in"]
    w["da_lambda"] = np.ascontiguousarray(inp["da_lambda"].reshape(DEPTH, 128))
    w["da_sublnT"] = np.ascontiguousarray(inp["da_subln"].reshape(DEPTH, 64, 1))
    w["gqT"] = np.ascontiguousarray(inp["mla_q_norm"].reshape(DEPTH, 2, 128).transpose(0, 2, 1))
    w["w_uq"] = inp["mla_w_uq"]
    w["gkvT"] = np.ascontiguousarray(inp["mla_kv_norm"].reshape(DEPTH, 128, 1))
    wk = inp["mla_w_ukv"].reshape(DEPTH, 128, 6, 128)
    w["w_ukv"] = np.ascontiguousarray(np.concatenate([wk[..., :64].reshape(DEPTH, 128, 384), wk[..., 64:].reshape(DEPTH, 128, 384)], -1))
    w["decay"] = np.ascontiguousarray(np.concatenate([inp["ret_decay_f"], inp["ret_decay_b"]], -1))
    w["w_out"] = inp["w_out"]
    for nm in ("ln1_g", "ln1_b", "ln2_g", "ln2_b"):
        w[nm + "T"] = np.ascontiguousarray(inp[nm].reshape(DEPTH, 8, 128).transpose(0, 2, 1))
    w["router_w"] = inp["router_w"]
    w["router_b"] = inp["router_b"]
    w["exp_w1"], w["exp_w3"], w["exp_w2"] = inp["exp_w1"], inp["exp_w3"], inp["exp_w2"]
    w["ropeA"], w["ropeM"], w["ropeR"] = rope_tables(r)
    w["tpos"] = np.ascontiguousarray((np.arange(32)[None, :] * 128 + np.arange(128)[:, None]).astype(f))
    BIG = 1.0e9
    eb = np.full((128, 16), BIG, f)
    for rp in range(4):
        if rp < r:
            eb[:, rp] = T * (r - 1 - rp)
        if rp > r:
            eb[:, 4 + rp] = T * (rp - r - 1)
    eb[:, 8] = T * r
    eb[:, 9] = T * (3 - r)
    w["ebound"] = eb
    w["ident"] = np.eye(128, dtype=f)
    mm = np.arange(128)[:, None].astype(f)
    ccc = np.arange(128)[None, :].astype(f)
    w["rtabs"] = np.ascontiguousarray(np.stack([np.maximum(ccc - mm, 0), np.maximum(mm - ccc, 0),
                                                 np.broadcast_to(ccc + 1, (128, 128)), np.broadcast_to(128 - ccc, (128, 128))], 0).astype(f))
    return w


PERLAYER_NAMES = ("w_ada", "b_adaT", "w_in", "da_lambda", "da_sublnT", "gqT", "w_uq", "gkvT", "w_ukv", "decay", "w_out",
                  "ln1_gT", "ln1_bT", "ln2_gT", "ln2_bT", "exp_w1", "exp_w3", "exp_w2")
STAGE_WEIGHTS = {
    "A": ("cvec", "w_ada", "b_adaT", "w_in", "gqT", "w_uq", "gkvT", "w_ukv", "decay", "ropeA", "ropeM", "ropeR", "tpos", "ident"),
    "X": ("ident",),
    "B": ("da_lambda", "da_sublnT", "ident"),
    "R": ("decay", "ebound", "tpos", "ident", "rtabs"),
    "C": ("cvec", "w_ada", "b_adaT", "w_out", "ln1_gT", "ln1_bT", "ln2_gT", "ln2_bT", "router_w", "router_b",
          "exp_w1", "exp_w3", "exp_w2", "ident"),
}
def stage_io(st, l):
    def xn(l_):
        if l_ == 0:
            return ["xT", "cxT"]
        if l_ == DEPTH:
            return ["xT_out"]
        return ["xTL%d" % l_, "cxTL%d" % l_]
    own = ["kda_own", "vda_own", "kml_own", "vml_own", "rsum_own"]
    gat = ["kda_all", "vda_all", "kml_all", "vml_all", "rsum_all"]
    if st == "A":
        return xn(l), ["qda", "qdaf", "qml", "qmlf", "qr", "kr", "krt", "vr", "sg"] + own
    if st == "X":
        return own, gat
    if st == "B":
        return ["qda", "qdaf", "kda_own", "vda_own", "qml", "qmlf", "kml_own", "vml_own", "kda_all", "vda_all", "kml_all", "vml_all"], ["yT"]
    if st == "R":
        return ["qr", "kr", "krt", "vr", "sg", "rsum_all", "yT"], ["yT"]
    return ["yT"] + xn(l), xn(l + 1)


NPDT = {"bf16": NPBF, "f32": np.float32}


def run_launch(stages, state, hw, keep=None):
    layers = sorted({l for _, l in stages})
    layer_of = {l: i for i, l in enumerate(layers)}
    wnames = []
    for st, _ in stages:
        for n in STAGE_WEIGHTS[st]:
            if n not in wnames:
                wnames.append(n)
    produced, ext_in, ext_out = set(), [], []
    for st, l_ in stages:
        rd, wr = stage_io(st, l_)
        for n in rd:
            if n not in produced and n not in ext_in:
                ext_in.append(n)
        for n in wr:
            produced.add(n)
            if n not in ext_out and n not in ext_in:
                ext_out.append(n)
    if keep is not None:
        ext_out = [n for n in ext_out if n in keep]
    in_maps = []
    wshapes = None
    for c in range(NCORE):
        m = {}
        for n in wnames:
            a = hw[c][n]
            if n in PERLAYER_NAMES:
                a = np.ascontiguousarray(a[layers])
            m[n] = a
        if wshapes is None:
            wshapes = {n: m[n].shape for n in wnames}
        for n in ext_in:
            m[n] = state[c][n]
        in_maps.append(m)
    nc = build_program(stages, layer_of, set(ext_in), set(ext_out), wshapes)
    res = run_bass_kernel_spmd(nc, in_maps, core_ids=list(range(NCORE)))
    for c in range(NCORE):
        for n in ext_out:
            state[c][n] = res.results[c][n]
    return res


FUSED = True


def kernel(**inputs):
    inp = {k: np.asarray(v) for k, v in inputs.items()}
    hw = [host_weights(inp, c) for c in range(NCORE)]
    state = []
    for c in range(NCORE):
        b, r = c // 4, c % 4
        xs = inp["x"][b, r * T:(r + 1) * T, :]
        state.append({"xT": np.ascontiguousarray(xs.T.reshape(8, 128, T)),
                      "cxT": np.ascontiguousarray(inp["ctx"][b].T.reshape(8, 128, CT))})
    allst = [(st, l) for l in range(DEPTH) for st in ("A", "X", "B", "R", "C")]
    if FUSED:
        run_launch(allst, state, hw, keep=("xT_out",))
    else:
        for l in range(DEPTH):
            run_launch([(st, l) for st in ("A", "X", "B", "R", "C")], state, hw,
                       keep=("xT_out", "xTL%d" % (l + 1), "cxTL%d" % (l + 1)))
    out = np.empty((2, S, D), np.float32)
    for c in range(NCORE):
        b, r = c // 4, c % 4
        out[b, r * T:(r + 1) * T, :] = state[c]["xT_out"].reshape(D, T).T
    return out
```
